# Optimizing a Trainium2 kernel written in Bass

```python
import jax, jax.numpy as jnp
from jax import lax
import numpy as np

D_MODEL = 1024
BATCH = 8
SEQ = 4096
DEPTH = 1

HEAD_DIM = 64
N_FOX_HEADS = 8
N_MOBA_HEADS = 8
FOX_WIDTH = N_FOX_HEADS * HEAD_DIM
MOBA_WIDTH = N_MOBA_HEADS * HEAD_DIM
Q_BLOCK = 128
MOBA_BLOCK = 256
MOBA_TOPK = 3
MOBA_Q_CHUNK = 32
ROPE_THETA = 10000.0
PLE_DIM = 256
N_GROUPS = 4
EXPERTS_PER_GROUP = 8
N_EXPERTS = N_GROUPS * EXPERTS_PER_GROUP
TOP_K_FINE = 2
D_EXPERT = 512
MOE_ROW_BLOCK = 128
RMS_EPS = 1e-6
IN_COLS = 3 * FOX_WIDTH + 3 * MOBA_WIDTH + N_FOX_HEADS + 2 * D_MODEL

kernel_name = 'fox_moba_hmoe_hybrid_block'


def rmsnorm(x, g):
    xf = x.astype(jnp.float32)
    y = xf * lax.rsqrt(jnp.mean(xf * xf, axis=-1, keepdims=True) + RMS_EPS)
    return (y * g.astype(jnp.float32)).astype(x.dtype)


def rope(x):
    s, d = x.shape[2], x.shape[3]
    half = d // 2
    inv = 1.0 / (ROPE_THETA ** (jnp.arange(0, d, 2, dtype=jnp.float32) / d))
    ang = jnp.arange(s, dtype=jnp.float32)[:, None] * inv[None, :]
    cos, sin = jnp.cos(ang), jnp.sin(ang)
    xf = x.astype(jnp.float32)
    x1, x2 = xf[..., :half], xf[..., half:]
    out = jnp.concatenate([x1 * cos - x2 * sin, x2 * cos + x1 * sin], axis=-1)
    return out.astype(x.dtype)


def forgetting_attention(q, k, v, log_f):
    b, h, s, d = q.shape
    c = jnp.cumsum(log_f, axis=-1)
    nqb = s // Q_BLOCK
    q_blocks = jnp.moveaxis(q.reshape(b, h, nqb, Q_BLOCK, d), 2, 0)
    c_blocks = jnp.moveaxis(c.reshape(b, h, nqb, Q_BLOCK), 2, 0)
    starts = jnp.arange(nqb, dtype=jnp.int32) * Q_BLOCK
    k_pos = jnp.arange(s, dtype=jnp.int32)
    scale = HEAD_DIM ** -0.5

    def block(args):
        qb, cb, s0 = args
        logits = jnp.einsum('bhqd,bhkd->bhqk', qb, k, preferred_element_type=jnp.float32) * scale
        logits = logits + cb[..., None] - c[:, :, None, :]
        q_pos = s0 + jnp.arange(Q_BLOCK, dtype=jnp.int32)
        causal = k_pos[None, :] <= q_pos[:, None]
        logits = jnp.where(causal, logits, -jnp.inf)
        probs = jax.nn.softmax(logits, axis=-1)
        return jnp.einsum('bhqk,bhkd->bhqd', probs.astype(v.dtype), v)

    out = lax.map(block, (q_blocks, c_blocks, starts))
    return jnp.moveaxis(out, 0, 2).reshape(b, h, s, d)


def moba_attention(q, k, v):
    b, h, s, d = q.shape
    nb = -(-s // MOBA_BLOCK)
    sp = nb * MOBA_BLOCK
    pad = sp - s
    q, k, v = [jnp.pad(t, ((0, 0), (0, 0), (0, pad), (0, 0))) for t in (q, k, v)]
    k_blocks = k.reshape(b, h, nb, MOBA_BLOCK, d)
    v_blocks = v.reshape(b, h, nb, MOBA_BLOCK, d)
    k_mean = jnp.mean(k_blocks.astype(jnp.float32), axis=3)
    gate = jnp.einsum('bhsd,bhnd->bhsn', q.astype(jnp.float32), k_mean)
    q_blk = jnp.arange(sp, dtype=jnp.int32) // MOBA_BLOCK
    past = jnp.arange(nb, dtype=jnp.int32)[None, :] < q_blk[:, None]
    gate = jnp.where(past, gate, -jnp.inf)
    n_sel = min(MOBA_TOPK, nb)
    _, sel = lax.top_k(gate, n_sel)
    sel_valid = sel < q_blk[:, None]

    nc = sp // MOBA_Q_CHUNK
    q_c = jnp.moveaxis(q.reshape(b, h, nc, MOBA_Q_CHUNK, d), 2, 0)
    sel_c = jnp.moveaxis(sel.reshape(b, h, nc, MOBA_Q_CHUNK, n_sel), 2, 0)
    val_c = jnp.moveaxis(sel_valid.reshape(b, h, nc, MOBA_Q_CHUNK, n_sel), 2, 0)
    starts = jnp.arange(nc, dtype=jnp.int32) * MOBA_Q_CHUNK
    gather = jax.vmap(jax.vmap(lambda blocks, ix: blocks[ix]))
    own_off = jnp.arange(MOBA_BLOCK, dtype=jnp.int32)
    scale = HEAD_DIM ** -0.5
    n_keys_sel = n_sel * MOBA_BLOCK

    def chunk(args):
        qc, sc, vc, s0 = args
        b0 = s0 // MOBA_BLOCK
        ks = gather(k_blocks, sc)
        vs = gather(v_blocks, sc)
        l_sel = jnp.einsum('bhqd,bhqnkd->bhqnk', qc, ks, preferred_element_type=jnp.float32) * scale
        l_sel = jnp.where(vc[..., None], l_sel, -jnp.inf).reshape(b, h, MOBA_Q_CHUNK, n_keys_sel)
        k_own = lax.dynamic_index_in_dim(k_blocks, b0, axis=2, keepdims=False)
        v_own = lax.dynamic_index_in_dim(v_blocks, b0, axis=2, keepdims=False)
        l_own = jnp.einsum('bhqd,bhkd->bhqk', qc, k_own, preferred_element_type=jnp.float32) * scale
        q_pos = s0 + jnp.arange(MOBA_Q_CHUNK, dtype=jnp.int32)
        k_pos = b0 * MOBA_BLOCK + own_off
        l_own = jnp.where(k_pos[None, :] <= q_pos[:, None], l_own, -jnp.inf)
        probs = jax.nn.softmax(jnp.concatenate([l_sel, l_own], axis=-1), axis=-1)
        p_sel = probs[..., :n_keys_sel].reshape(b, h, MOBA_Q_CHUNK, n_sel, MOBA_BLOCK)
        p_own = probs[..., n_keys_sel:]
        return (jnp.einsum('bhqnk,bhqnkd->bhqd', p_sel.astype(v.dtype), vs)
                + jnp.einsum('bhqk,bhkd->bhqd', p_own.astype(v.dtype), v_own))

    out = lax.map(chunk, (q_c, sel_c, val_c, starts))
    return jnp.moveaxis(out, 0, 2).reshape(b, h, sp, d)[:, :, :s]


def hierarchical_moe(h, w_group, b_group, w_fine, b_fine, w_gate, w_up, w_down):
    b, s, dm = h.shape
    t = b * s
    xt = h.reshape(t, dm)
    xf = xt.astype(jnp.float32)
    g_prob = jax.nn.softmax(xf @ w_group.astype(jnp.float32) + b_group.astype(jnp.float32), axis=-1)
    g_top, g_idx = lax.top_k(g_prob, 1)
    f_logits = (xf @ w_fine.astype(jnp.float32) + b_fine.astype(jnp.float32)).reshape(t, N_GROUPS, EXPERTS_PER_GROUP)
    f_logits = jnp.take_along_axis(f_logits, g_idx[:, :, None], axis=1)[:, 0]
    f_top, f_idx = lax.top_k(f_logits, TOP_K_FINE)
    weights = (g_top * jax.nn.softmax(f_top, axis=-1)).astype(h.dtype)
    expert_idx = g_idx * EXPERTS_PER_GROUP + f_idx

    n_assign = t * TOP_K_FINE
    flat_e = expert_idx.reshape(-1).astype(jnp.int32)
    flat_w = weights.reshape(-1)
    flat_t = jnp.repeat(jnp.arange(t, dtype=jnp.int32), TOP_K_FINE)
    order = jnp.argsort(flat_e)
    se, st, sw = flat_e[order], flat_t[order], flat_w[order]
    counts = jnp.bincount(flat_e, length=N_EXPERTS)
    starts = jnp.cumsum(counts) - counts
    padded = ((counts + MOE_ROW_BLOCK - 1) // MOE_ROW_BLOCK) * MOE_ROW_BLOCK
    pends = jnp.cumsum(padded)
    pstarts = pends - padded
    dest = pstarts[se] + (jnp.arange(n_assign, dtype=jnp.int32) - starts[se])
    n_rows = n_assign + N_EXPERTS * MOE_ROW_BLOCK
    n_blk = n_rows // MOE_ROW_BLOCK
    row_token = jnp.full((n_rows,), t, dtype=jnp.int32).at[dest].set(st)
    row_weight = jnp.zeros((n_rows,), dtype=h.dtype).at[dest].set(sw)
    block_start = jnp.arange(n_blk, dtype=jnp.int32) * MOE_ROW_BLOCK
    block_expert = jnp.minimum(jnp.searchsorted(pends, block_start, side='right'), N_EXPERTS - 1)
    x_pad = jnp.concatenate([xt, jnp.zeros((1, dm), xt.dtype)], axis=0)
    rows = x_pad[row_token].reshape(n_blk, MOE_ROW_BLOCK, dm)

    def expert_block(args):
        r, e = args
        return (jax.nn.silu(r @ w_gate[e]) * (r @ w_up[e])) @ w_down[e]

    out_rows = lax.map(expert_block, (rows, block_expert)).reshape(n_rows, dm)
    out = jax.ops.segment_sum(out_rows * row_weight[:, None], row_token, num_segments=t + 1)[:t]
    return out.reshape(b, s, dm)


def setup_inputs(seed: int = 0) -> dict:
    key = jax.random.key(seed)
    ks = jax.random.split(key, 24)
    f32 = jnp.float32

    def nrm(k, shape, fan_in):
        return jax.random.normal(k, shape, f32) * (fan_in ** -0.5)

    def gain(k, shape):
        return 1.0 + 0.01 * jax.random.normal(k, shape, f32)

    return {
        'x': jax.random.normal(ks[0], (BATCH, SEQ, D_MODEL), f32),
        'p': jax.random.normal(ks[1], (DEPTH, BATCH, SEQ, PLE_DIM), f32),
        'attn_norm': gain(ks[2], (DEPTH, D_MODEL)),
        'w_in': nrm(ks[3], (DEPTH, D_MODEL, IN_COLS), D_MODEL),
        'b_forget': 0.1 * jax.random.normal(ks[4], (DEPTH, N_FOX_HEADS), f32),
        'w_fox_branch': nrm(ks[5], (DEPTH, FOX_WIDTH, D_MODEL), FOX_WIDTH),
        'w_moba_branch': nrm(ks[6], (DEPTH, MOBA_WIDTH, D_MODEL), MOBA_WIDTH),
        'w_out': nrm(ks[7], (DEPTH, D_MODEL, D_MODEL), D_MODEL),
        'moe_norm': gain(ks[8], (DEPTH, D_MODEL)),
        'w_group': nrm(ks[9], (DEPTH, D_MODEL, N_GROUPS), D_MODEL),
        'b_group': 0.01 * jax.random.normal(ks[10], (DEPTH, N_GROUPS), f32),
        'w_fine': nrm(ks[11], (DEPTH, D_MODEL, N_EXPERTS), D_MODEL),
        'b_fine': 0.01 * jax.random.normal(ks[12], (DEPTH, N_EXPERTS), f32),
        'w_gate': nrm(ks[13], (DEPTH, N_EXPERTS, D_MODEL, D_EXPERT), D_MODEL),
        'w_up': nrm(ks[14], (DEPTH, N_EXPERTS, D_MODEL, D_EXPERT), D_MODEL),
        'w_down': nrm(ks[15], (DEPTH, N_EXPERTS, D_EXPERT, D_MODEL), D_EXPERT),
        'ple_norm': gain(ks[16], (DEPTH, D_MODEL)),
        'w_ple_gate': nrm(ks[17], (DEPTH, D_MODEL, D_MODEL), D_MODEL),
        'w_ple_proj': nrm(ks[18], (DEPTH, PLE_DIM, D_MODEL), PLE_DIM),
        'final_norm': gain(ks[19], (D_MODEL,)),
    }


def reference(x, p, attn_norm, w_in, b_forget, w_fox_branch, w_moba_branch, w_out,
              moe_norm, w_group, b_group, w_fine, b_fine, w_gate, w_up, w_down,
              ple_norm, w_ple_gate, w_ple_proj, final_norm):
    b, s, _ = x.shape
    split_at = [int(v) for v in np.cumsum([FOX_WIDTH] * 3 + [MOBA_WIDTH] * 3 + [N_FOX_HEADS, D_MODEL])]

    def heads(t, n):
        return t.reshape(b, s, n, HEAD_DIM).transpose(0, 2, 1, 3)

    def merge(t):
        return t.transpose(0, 2, 1, 3).reshape(b, s, -1)

    for i in range(DEPTH):
        h = rmsnorm(x, attn_norm[i])
        proj = jnp.einsum('bsd,dc->bsc', h, w_in[i])
        qf, kf, vf, qm, km, vm, f_logit, ga, gb = jnp.split(proj, split_at, axis=-1)
        log_f = jax.nn.log_sigmoid(f_logit.astype(jnp.float32) + b_forget[i].astype(jnp.float32))
        y_fox = forgetting_attention(heads(qf, N_FOX_HEADS), heads(kf, N_FOX_HEADS),
                                     heads(vf, N_FOX_HEADS), log_f.transpose(0, 2, 1))
        y_moba = moba_attention(rope(heads(qm, N_MOBA_HEADS)), rope(heads(km, N_MOBA_HEADS)),
                                heads(vm, N_MOBA_HEADS))
        ya = merge(y_fox) @ w_fox_branch[i]
        yb = merge(y_moba) @ w_moba_branch[i]
        mixed = jax.nn.sigmoid(ga) * ya + jax.nn.sigmoid(gb) * yb
        x = x + mixed @ w_out[i]
        h2 = rmsnorm(x, moe_norm[i])
        x = x + hierarchical_moe(h2, w_group[i], b_group[i], w_fine[i], b_fine[i],
                                 w_gate[i], w_up[i], w_down[i])
        g = jax.nn.sigmoid(rmsnorm(x, ple_norm[i]) @ w_ple_gate[i])
        x = x + g * (p[i] @ w_ple_proj[i])
    return rmsnorm(x, final_norm)
```

```python
import numpy as np
from contextlib import ExitStack
import concourse.bass as bass
import concourse.mybir as mybir
from concourse.bass_utils import run_bass_kernel_spmd
import ml_dtypes

F32 = mybir.dt.float32
BF16 = mybir.dt.bfloat16
I32 = mybir.dt.int32
AF = mybir.ActivationFunctionType
ALU = mybir.AluOpType
AX = mybir.AxisListType

S = 4096
D = 1024
NT = S // 128
NG = S // 512
NH = 8
HD = 64
KF = 67
KM = 80
NEG = -30000.0
NE = 32
DE = 512
NSLOT = 48
SLOT = 512
NJ = S // SLOT
PLE = 256
EPS = 1e-6


class Prog:
    def __init__(self, nc, es):
        self.nc, self.es = nc, es
        self.eng = {'pe': nc.tensor, 'act': nc.scalar, 'dve': nc.vector, 'pool': nc.gpsimd, 'sp': nc.sync}
        self.ops = []
        self.emitted = 0
        self.wr = {}
        self.rd = {}
        self.prd = {}
        self.cur = {}
        self.dsem = {}
        self.done = {}
        self.waited = {}
        self.nsem = 0
        self.allsems = []
        self.freed = {'sp': [], 'pool': [], 'act': []}
        self.dsem_eng = {}

    def newsem(self, name):
        self.nsem += 1
        s = self.es.enter_context(self.nc.semaphore(f"{name}_{self.nsem}"))
        return s

    def op(self, eng, fn, r=(), w=(), dma=None, waw=True):
        i = len(self.ops)
        deps = set()
        for x in r:
            deps.update(self.wr.get(x, ()))
            self.rd.setdefault(x, []).append(i)
        for x in w:
            rds = self.rd.get(x, [])
            if rds:
                deps.update(rds)
                deps.update(self.wr.get(x, ()))
                self.wr[x] = [i]
                self.prd[x] = rds
                self.rd[x] = []
            elif waw:
                deps.update(self.wr.get(x, ()))
                deps.update(self.prd.get(x, ()))
                self.wr[x] = [i]
            else:
                deps.update(self.prd.get(x, ()))
                self.wr.setdefault(x, []).append(i)
        deps.discard(i)
        self.ops.append([eng, fn, deps, dma])
        return i

    def _skip(self, d, eng):
        deng, _, _, ddma = self.ops[d]
        return ddma is None and deng == 'pe' and eng == 'pe'

    def flush(self, barrier=True):
        n = len(self.ops)
        needed = set()
        for i in range(self.emitted, n):
            eng, fn, deps, dma = self.ops[i]
            for d in deps:
                if not self._skip(d, eng):
                    needed.add(d)
        last = {}
        for i in range(self.emitted, n):
            if self.ops[i][3] is None:
                last[self.ops[i][0]] = i
        needed.update(last.values())
        for lst in list(self.wr.values()) + list(self.rd.values()) + list(self.prd.values()):
            needed.update(lst)
        for i in range(self.emitted, n):
            eng, fn, deps, dma = self.ops[i]
            E = self.eng[eng]
            need = {}
            for d in deps:
                if self._skip(d, eng):
                    continue
                sem, val, dent = self.done[d]
                if dent is not None:
                    val = dent[1]
                k = id(sem)
                if need.get(k, (None, 0))[1] < val:
                    need[k] = (sem, val)
            for k, (sem, val) in need.items():
                if self.waited.get((eng, k), 0) >= val:
                    continue
                E.wait_ge(sem, val)
                self.waited[(eng, k)] = val
            ins = fn()
            if dma is not None:
                ent = self.dsem.get(dma)
                if ent is None:
                    self.dsem_eng[dma] = eng
                    if self.freed[eng]:
                        ent = self.dsem[dma] = self.freed[eng].pop()
                    else:
                        ent = self.dsem[dma] = [self.newsem('d'), 0]
                        self.allsems.append(ent)
                ent[1] += 16
                ins.then_inc(ent[0], 16)
                self.done[i] = (ent[0], ent[1], ent)
            elif i in needed:
                ent = self.cur.get(eng)
                if ent is None or ent[1] >= 6000:
                    ent = self.cur[eng] = [self.newsem('e' + eng), 0]
                    self.allsems.append(ent)
                ent[1] += 1
                ins.then_inc(ent[0], 1)
                self.done[i] = (ent[0], ent[1], None)
            self.ops[i][1] = None
        self.emitted = n
        if barrier:
            for eng, E in self.eng.items():
                for ent in self.allsems:
                    if ent[1] == 0:
                        continue
                    k = id(ent[0])
                    if self.waited.get((eng, k), 0) >= ent[1]:
                        continue
                    E.wait_ge(ent[0], ent[1])
                    self.waited[(eng, k)] = ent[1]
            self.wr = {}
            self.rd = {}
            self.prd = {}
            for k_, ent in self.dsem.items():
                self.freed[self.dsem_eng[k_]].append(ent)
            self.dsem = {}


def build(debug=(), stop=None):
    nc = bass.Bass("TRN2", target_bir_lowering=False)
    dbg = set(debug)

    def dram_in(name, shape, dt=F32):
        return nc.dram_tensor(name, list(shape), dt, kind="ExternalInput")

    def scratch(name, shape, dt):
        return nc.dram_tensor(name, list(shape), dt, kind=("ExternalOutput" if name in dbg else "Internal"))

    x_d = dram_in("x", [S, D])
    p_d = dram_in("p", [S, PLE])
    wfm_d = dram_in("w_fm", [40, 128, 8, 128])
    wtm_d = dram_in("w_tm", [2, 128, 8, 512])
    wfl_d = dram_in("w_fl", [128, 8, 8])
    g1_d = dram_in("attn_norm", [1, D])
    bf_d = dram_in("b_forget", [8, 1])
    cs1_d = dram_in("cs1", [128, S])
    cs2_d = dram_in("cs2", [128, S])
    id_d = dram_in("ident", [128, 128])
    out_d = nc.dram_tensor("out", [S, D], F32, kind="ExternalOutput")

    qf_s = scratch("qf_s", [NH, KF, S], BF16)
    kf_s = scratch("kf_s", [NH, KF, S], BF16)
    vf_s = scratch("vf_s", [NH, 128, NT, 65], BF16)
    qm_s = scratch("qm_s", [NH, KM, S], BF16)
    km_s = scratch("km_s", [NH, KM, S], BF16)
    vm_s = scratch("vm_s", [NH, 128, NT, 65], BF16)
    sga_s = scratch("sga_s", [8, 128, S], BF16)
    sgb_s = scratch("sgb_s", [8, 128, S], BF16)
    cneg_s = scratch("cneg_s", [8, S], F32)
    ccol_s = scratch("ccol_s", [128, NT * 8], F32)
    yf_s = scratch("yf_s", [512, S], BF16)
    ym_s = scratch("ym_s", [512, S], BF16)
    cmask_d = dram_in("cmask", [128, 4, 512])
    wfb_d = dram_in("w_fb", [128, 4, D])
    wmb_d = dram_in("w_mb", [128, 4, D])
    wo_d = dram_in("w_o", [128, 8, D])
    g2_d = dram_in("moe_norm", [1, D])
    wr_d = dram_in("w_r", [128, 8, 36])
    br_d = dram_in("b_r", [1, 36])
    ustr_d = dram_in("ustrict", [128, 128])
    jthr_d = dram_in("jthr", [1, NSLOT])
    pid_d = dram_in("pid", [128, 1])
    if stop in (None, 'E2'):
        wg_d = dram_in("w_g", [NE * 128, 8 * DE])
        wu_d = dram_in("w_u", [NE * 128, 8 * DE])
        wd_d = dram_in("w_d", [NE * 128, 4 * D])
    g3_d = dram_in("ple_norm", [1, D])
    g4_d = dram_in("final_norm", [1, D])
    wpg_d = dram_in("w_pg", [128, 8, D])
    wpp_d = dram_in("w_pp", [128, 2, D])
    x1_s = scratch("x1_s", [S, D], F32)
    h2_s = scratch("h2_s", [S, D], BF16)
    xs_s = scratch("xs_s", [NSLOT * SLOT, D], BF16)
    or_s = scratch("or_s", [NSLOT * SLOT, D], F32)
    dbgI_s = scratch("dbgI_s", [128, 2 * NT + NSLOT], I32)
    blk1h_d = dram_in("blk1h", [16, S])

    class _Stop(Exception):
        pass

    try:
      with ExitStack() as es:
        P = Prog(nc, es)

        def chk(tag):
            if stop == tag:
                P.flush(barrier=True)
                raise _Stop()
        V, A, T, G, SP = 'dve', 'act', 'pe', 'pool', 'sp'
        ve, ac, pe, gp, sp = nc.vector, nc.scalar, nc.tensor, nc.gpsimd, nc.sync

        bc_reg = nc.gpsimd.to_reg(NE * 128 - 1)

        def sb(stack, name, shape, dt):
            return stack.enter_context(nc.sbuf_tensor("sb_" + name, list(shape), dt))

        ps = [es.enter_context(nc.psum_tensor(f"ps{i}", [128, 512], F32)) for i in range(8)]
        ident_f = sb(es, "ident_f", [128, 128], F32)
        ident_b = sb(es, "ident_b", [128, 128], BF16)
        P.op(SP, lambda: sp.dma_start(out=ident_f[:], in_=id_d[:, :]), w=['ident_f'], dma='ident_f')
        P.op(V, lambda: ve.tensor_copy(out=ident_b[:], in_=ident_f[:]), r=['ident_f'], w=['ident_b'])
        ksum = sb(es, "ksum", [128, 4, 16], F32)
        cmask = sb(es, "cmask", [128, 4, 512], BF16)
        P.op(G, lambda: gp.dma_start(out=cmask[:], in_=cmask_d[:, :, :]), w=['cmask'], dma='cmask')

        zt = sb(es, "zt", [128, 4, D], BF16)
        P.op(G, lambda: gp.memset(zt[:], 0.0), w=['zt'])
        for a in range(NSLOT):
            P.op(G, lambda a=a: gp.dma_start(out=xs_s[a * SLOT:(a + 1) * SLOT, :].rearrange("(a p) d -> p a d", p=128), in_=zt[:]),
                 r=['zt'], w=['xs_zero'], dma='zt_st', waw=False)

        with ExitStack() as ph:
            hT = sb(ph, "hT", [128, 8, S], BF16)
            ph1 = ExitStack()
            g1 = sb(ph1, "g1", [128, D], F32)
            xt = [sb(ph1, f"xt{i}", [128, D], F32) for i in range(2)]
            hb = [sb(ph1, f"hb{i}", [128, D], BF16) for i in range(2)]
            sq = sb(ph1, "sq", [128, D], BF16)
            ss = [sb(ph1, f"ss{i}", [128, 1], F32) for i in range(2)]
            rs = [sb(ph1, f"rs{i}", [128, 1], F32) for i in range(2)]
            P.op(SP, lambda: sp.dma_start(out=g1[:], in_=g1_d[:, :].partition_broadcast(128)), w=['g1'], dma='g1')
            a1_pending = []

            def a1_copy(t, b, pb):
                P.op(A, lambda: ac.activation(out=hT[:, :, t * 128:(t + 1) * 128], in_=pb.rearrange("p (k c) -> p k c", k=8), func=AF.Copy),
                     r=[f'ps{b}'], w=[f'hT{t // 4}'], waw=False)

            for t in range(NT):
                b = t % 2
                P.op(SP, lambda t=t, b=b: sp.dma_start(out=xt[b][:], in_=x_d[t * 128:(t + 1) * 128, :]),
                     w=[f'xt{b}'], dma=f'xt{b}')
                P.op(A, lambda b=b: ac.activation(out=sq[:], in_=xt[b][:], func=AF.Square, accum_out=ss[b][:]),
                     r=[f'xt{b}'], w=['sq', f'ss{b}'])
                P.op(A, lambda b=b: ac.activation(out=rs[b][:], in_=ss[b][:], func=AF.Ln, scale=1.0 / D, bias=EPS),
                     r=[f'ss{b}'], w=[f'rs{b}'])
                P.op(A, lambda b=b: ac.activation(out=rs[b][:], in_=rs[b][:], func=AF.Exp, scale=-0.5),
                     r=[f'rs{b}'], w=[f'rs{b}'])
                P.op(V, lambda b=b: ve.scalar_tensor_tensor(out=hb[b][:], in0=xt[b][:], scalar=rs[b][:, 0:1], in1=g1[:],
                                                            op0=ALU.mult, op1=ALU.mult),
                     r=[f'xt{b}', f'rs{b}', 'g1'], w=[f'hb{b}'])
                pb = ps[b][:].bitcast(BF16)
                for kc in range(8):
                    P.op(T, lambda b=b, kc=kc, pb=pb: pe.transpose(out=pb[:, kc * 128:(kc + 1) * 128],
                                                                 in_=hb[b][:, kc * 128:(kc + 1) * 128], identity=ident_b[:]),
                         r=[f'hb{b}', 'ident_b'], w=[f'ps{b}'], waw=(kc == 0))
                a1_pending.append((t, b, pb))
                if len(a1_pending) > 1:
                    a1_copy(*a1_pending.pop(0))
            while a1_pending:
                a1_copy(*a1_pending.pop(0))
            P.flush(barrier=True)
            ph1.close()
            chk('A1')

            with ExitStack() as ph2:
                cs1 = sb(ph2, "cs1", [128, S], F32)
                cs2 = sb(ph2, "cs2", [128, S], F32)
                P.op(SP, lambda: sp.dma_start(out=cs1[:], in_=cs1_d[:, :]), w=['cs1'], dma='cs1')
                P.op(SP, lambda: sp.dma_start(out=cs2[:], in_=cs2_d[:, :]), w=['cs2'], dma='cs2')
                wb = [sb(ph2, f"wb{i}", [128, 8, 128], BF16) for i in range(4)]
                wv = [sb(ph2, f"wv{i}", [128, 8, 512], BF16) for i in range(2)]
                wfl = sb(ph2, "wfl", [128, 8, 8], BF16)
                bfg = sb(ph2, "bfg", [8, 1], F32)
                stg = [sb(ph2, f"stg{i}", [128, 512], BF16) for i in range(4)]
                t1 = [sb(ph2, f"t1_{i}", [128, 512], F32) for i in range(2)]
                t2 = [sb(ph2, f"t2_{i}", [128, 512], F32) for i in range(2)]
                vst = [sb(ph2, f"vst{i}", [128, 8, 65], BF16) for i in range(2)]
                ones8 = sb(ph2, "ones8", [8, 512], F32)
                onesb = sb(ph2, "onesb", [8, 512], BF16)
                sp8 = sb(ph2, "sp8", [8, 512], F32)
                cn8 = sb(ph2, "cn8", [8, S], F32)
                c_hi = sb(ph2, "c_hi", [8, 512], BF16)
                c_mid = sb(ph2, "c_mid", [8, 512], BF16)
                c_lo = sb(ph2, "c_lo", [8, 512], BF16)
                c_r = sb(ph2, "c_r", [8, 512], F32)
                c_t = sb(ph2, "c_t", [8, 512], F32)
                nstg = [0]
                psn = [0]

                def next_ps():
                    i = 2 + (psn[0] % 6)
                    psn[0] += 1
                    return i

                P.op(V, lambda: ve.memset(ones8[:], 1.0), w=['ones8'])
                P.op(V, lambda: ve.memset(onesb[:], 1.0), w=['onesb'])
                for i in range(2):
                    P.op(V, lambda i=i: ve.memset(vst[i][:], 1.0), w=[f'vst{i}'])
                P.op(SP, lambda: sp.dma_start(out=bfg[:], in_=bf_d[:, :]), w=['bfg'], dma='bfg')
                P.op(G, lambda: gp.dma_start(out=wfl[:], in_=wfl_d[:, :, :]), w=['wfl'], dma='wfl')
                for j in range(3):
                    for g in range(NG):
                        P.op(SP, lambda j=j, g=g: sp.dma_start(out=kf_s[:, 64 + j, g * 512:(g + 1) * 512], in_=onesb[:]),
                             r=['onesb'], w=[f'kfaug{j}_{g}'], dma='onesb_st')

                chk('A2a0')
                for g in range(NG):
                    pi = next_ps()
                    sl = slice(g * 512, (g + 1) * 512)
                    for kc in range(8):
                        P.op(T, lambda g=g, kc=kc, pi=pi: pe.matmul(ps[pi][0:8, :], lhsT=wfl[:, kc, :],
                                                                     rhs=hT[:, kc, g * 512:(g + 1) * 512],
                                                                     start=(kc == 0), stop=(kc == 7)),
                             r=['wfl', f'hT{g}'], w=[f'ps{pi}'], waw=(kc == 0))
                    P.op(V, lambda pi=pi: ve.tensor_scalar(out=sp8[:], in0=ps[pi][0:8, :],
                                                           scalar1=bfg[:, 0:1], scalar2=-1.0, op0=ALU.add, op1=ALU.mult),
                         r=[f'ps{pi}', 'bfg'], w=['sp8'])
                    P.op(A, lambda: ac.activation(out=sp8[:], in_=sp8[:], func=AF.Exp), r=['sp8'], w=['sp8'])
                    P.op(A, lambda: ac.activation(out=sp8[:], in_=sp8[:], func=AF.Ln, bias=1.0), r=['sp8'], w=['sp8'])
                    P.op(V, lambda g=g, sl=sl: ve.tensor_tensor_scan(out=cn8[:, sl], data0=ones8[:], data1=sp8[:],
                                                                     initial=(0.0 if g == 0 else cn8[:, g * 512 - 1:g * 512]),
                                                                     op0=ALU.mult, op1=ALU.add),
                         r=['sp8', 'ones8'] + ([f'cn8_{g - 1}'] if g else []), w=[f'cn8_{g}'])
                    if g == 0:
                        chk('A2a1')
                    P.op(V, lambda sl=sl: ve.tensor_scalar(out=c_t[:], in0=cn8[:, sl], scalar1=-1.0, scalar2=None, op0=ALU.mult),
                         r=[f'cn8_{g}'], w=['c_t'])
                    P.op(V, lambda: ve.tensor_copy(out=c_hi[:], in_=c_t[:]), r=['c_t'], w=['c_hi'])
                    P.op(V, lambda: ve.tensor_tensor(out=c_r[:], in0=c_t[:], in1=c_hi[:], op=ALU.subtract),
                         r=['c_t', 'c_hi'], w=['c_r'])
                    P.op(V, lambda: ve.tensor_copy(out=c_mid[:], in_=c_r[:]), r=['c_r'], w=['c_mid'])
                    P.op(V, lambda: ve.tensor_tensor(out=c_t[:], in0=c_r[:], in1=c_mid[:], op=ALU.subtract),
                         r=['c_r', 'c_mid'], w=['c_t'])
                    P.op(V, lambda: ve.tensor_copy(out=c_lo[:], in_=c_t[:]), r=['c_t'], w=['c_lo'])
                    for j, (nm, tl) in enumerate((('c_hi', c_hi), ('c_mid', c_mid), ('c_lo', c_lo))):
                        P.op(SP, lambda j=j, tl=tl, sl=sl: sp.dma_start(out=qf_s[:, 64 + j, sl], in_=tl[:]), r=[nm],
                             w=[f'qfaug{j}_{g}'], dma=f'{nm}_st')
                    if g == 0:
                        chk('A2a2')
                allc = [f'cn8_{g}' for g in range(NG)]
                P.op(SP, lambda: sp.dma_start(out=cneg_s[:, :], in_=cn8[:]), r=allc, w=['cneg_s'], dma='cn8_st')

                ccol = sb(ph2, "ccol", [128, NT * 8], F32)
                pcc = next_ps()
                for t in range(NT):
                    P.op(T, lambda t=t: pe.transpose(out=ps[pcc][:, t * 8:(t + 1) * 8], in_=cn8[:, t * 128:(t + 1) * 128],
                                                     identity=ident_f[0:8, 0:8]),
                         r=allc + ['ident_f'], w=[f'ps{pcc}'], waw=(t == 0))
                P.op(V, lambda: ve.tensor_copy(out=ccol[:], in_=ps[pcc][:, 0:NT * 8]), r=[f'ps{pcc}'], w=['ccol'])
                P.op(SP, lambda: sp.dma_start(out=ccol_s[:, :], in_=ccol[:]), r=['ccol'], w=['ccol_s'], dma='ccol_st')
                chk('A2a')
                nwb = [0]

                def load_w(blk):
                    i = nwb[0] % 4
                    nwb[0] += 1
                    P.op(G, lambda: gp.dma_start(out=wb[i][:], in_=wfm_d[blk, :, :, :]), w=[f'wb{i}'], dma=f'wb{i}')
                    return i

                def mm_block(wi, g, pi):
                    for kc in range(8):
                        P.op(T, lambda kc=kc: pe.matmul(ps[pi][:], lhsT=wb[wi][:, kc, :], rhs=hT[:, kc, g * 512:(g + 1) * 512],
                                                        start=(kc == 0), stop=(kc == 7)),
                             r=[f'wb{wi}', f'hT{g}'], w=[f'ps{pi}'], waw=(kc == 0))

                def store_pair(si, dst, pair, g):
                    for hh in range(2):
                        P.op(SP, lambda hh=hh: sp.dma_start(out=dst[2 * pair + hh, 0:64, g * 512:(g + 1) * 512],
                                                            in_=stg[si][hh * 64:(hh + 1) * 64, :]),
                             r=[f'stg{si}'], w=[f'{dst.name}_{pair}_{g}_{hh}'], dma=f'stg{si}_st')

                for blk in range(8):
                    wi = load_w(blk)
                    for g in range(NG):
                        pi = next_ps()
                        mm_block(wi, g, pi)
                        si = nstg[0] % 4
                        nstg[0] += 1
                        P.op(A, lambda pi=pi, si=si, blk=blk: ac.activation(out=stg[si][:], in_=ps[pi][:], func=AF.Identity,
                                                                            scale=(0.125 if blk < 4 else 1.0)),
                             r=[f'ps{pi}'], w=[f'stg{si}'])
                        store_pair(si, qf_s if blk < 4 else kf_s, blk % 4, g)
                chk('A2b')
                for (b0, dst, isk) in ((16, km_s, True), (8, qm_s, False)):
                    for pair in range(4):
                        w0 = load_w(b0 + pair)
                        w1 = load_w(b0 + 4 + pair)
                        for g in range(NG):
                            pa = next_ps()
                            mm_block(w0, g, pa)
                            pb_ = next_ps()
                            mm_block(w1, g, pb_)
                            ti = g % 2
                            sl = slice(g * 512, (g + 1) * 512)
                            P.op(V, lambda pa=pa, ti=ti, sl=sl: ve.tensor_tensor(out=t1[ti][:], in0=ps[pa][:], in1=cs1[:, sl], op=ALU.mult),
                                 r=[f'ps{pa}', 'cs1'], w=[f't1_{ti}'])
                            P.op(V, lambda pb_=pb_, ti=ti, sl=sl: ve.tensor_tensor(out=t2[ti][:], in0=ps[pb_][:], in1=cs2[:, sl], op=ALU.mult),
                                 r=[f'ps{pb_}', 'cs2'], w=[f't2_{ti}'])
                            si = nstg[0] % 4
                            nstg[0] += 1
                            if isk:
                                P.op(V, lambda ti=ti: ve.tensor_tensor(out=t1[ti][:], in0=t1[ti][:], in1=t2[ti][:], op=ALU.add),
                                     r=[f't1_{ti}', f't2_{ti}'], w=[f't1_{ti}'])
                                P.op(A, lambda ti=ti, si=si: ac.activation(out=stg[si][:], in_=t1[ti][:], func=AF.Copy),
                                     r=[f't1_{ti}'], w=[f'stg{si}'])
                                P.op(V, lambda ti=ti, pair=pair, g=g: ve.tensor_reduce(
                                    out=ksum[:, pair, 2 * g:2 * g + 2], in_=t1[ti][:].rearrange("p (b s) -> p b s", b=2),
                                    axis=AX.X, op=ALU.add), r=[f't1_{ti}'], w=[f'ksum_{pair}_{g}'])
                            else:
                                P.op(V, lambda ti=ti, si=si: ve.tensor_tensor(out=stg[si][:], in0=t1[ti][:], in1=t2[ti][:], op=ALU.add),
                                     r=[f't1_{ti}', f't2_{ti}'], w=[f'stg{si}'])
                            store_pair(si, dst, pair, g)
                chk('A2c')
                for blk in range(24, 40):
                    wi = load_w(blk)
                    dst = sga_s if blk < 32 else sgb_s
                    ch = (blk - 24) % 8
                    for g in range(NG):
                        pi = next_ps()
                        mm_block(wi, g, pi)
                        si = nstg[0] % 4
                        nstg[0] += 1
                        P.op(A, lambda pi=pi, si=si: ac.activation(out=stg[si][:], in_=ps[pi][:], func=AF.Sigmoid),
                             r=[f'ps{pi}'], w=[f'stg{si}'])
                        P.op(SP, lambda si=si, dst=dst, ch=ch, g=g: sp.dma_start(out=dst[ch, :, g * 512:(g + 1) * 512], in_=stg[si][:]),
                             r=[f'stg{si}'], w=[f'{dst.name}_{ch}_{g}'], dma=f'stg{si}_st')
                chk('A2d')
                nv = 0
                for vi, dst in enumerate((vf_s, vm_s)):
                    P.op(G, lambda vi=vi: gp.dma_start(out=wv[vi][:], in_=wtm_d[vi, :, :, :]), w=[f'wv{vi}'], dma=f'wv{vi}')
                    for t in range(NT):
                        pi = next_ps()
                        for kc in range(8):
                            P.op(T, lambda kc=kc, t=t, pi=pi, vi=vi: pe.matmul(ps[pi][:], lhsT=hT[:, kc, t * 128:(t + 1) * 128],
                                                                              rhs=wv[vi][:, kc, :], start=(kc == 0), stop=(kc == 7)),
                                 r=[f'wv{vi}', f'hT{t // 4}'], w=[f'ps{pi}'], waw=(kc == 0))
                        vb = nv % 2
                        nv += 1
                        P.op(A, lambda pi=pi, vb=vb: ac.activation(out=vst[vb][:, :, 0:64],
                                                                   in_=ps[pi][:].rearrange("p (h c) -> p h c", h=8), func=AF.Copy),
                             r=[f'ps{pi}'], w=[f'vst{vb}'])
                        P.op(SP, lambda vb=vb, dst=dst, t=t: sp.dma_start(out=dst[:, :, t, :].rearrange("h p c -> p h c"), in_=vst[vb][:]),
                             r=[f'vst{vb}'], w=[f'{dst.name}_{t}'], dma=f'vst{vb}_st')
                P.flush(barrier=True)

        chk('A2')
        with ExitStack() as ph:
            b1h = sb(ph, "b1h", [16, S], BF16)
            P.op(G, lambda: gp.dma_start(out=b1h[:], in_=blk1h_d[:, :]), w=['b1h'], dma='b1h')
            for h in range(NH):
                P.op(SP, lambda h=h: sp.dma_start(out=km_s[h, 64:80, :], in_=b1h[:]), r=['b1h'], w=[f'kmaug{h}'], dma='b1h_st')
            ksb = sb(ph, "ksb", [128, 4, 16], BF16)
            P.op(V, lambda: ve.tensor_copy(out=ksb[:], in_=ksum[:]), r=[f'ksum_{p_}_{g_}' for p_ in range(4) for g_ in range(NG)], w=['ksb'])
            qp = [sb(ph, f"qp{i}", [64, 2, 512], BF16) for i in range(4)]
            ksbB = sb(ph, "ksbB", [64, 4, 16], BF16)
            P.op(SP, lambda: sp.dma_start(out=ksbB[:], in_=ksb[64:128, :, :]), r=['ksb'], w=['ksbB'], dma='ksbB')
            gsb = sb(ph, "gsb", [128, 4, 8, 16], F32)
            sbt = sb(ph, "sbt", [128, 4, 8, 16], F32)
            sbb = sb(ph, "sbb", [128, 4, 128], BF16)
            tmp = sb(ph, "tmp", [128, 8, 16], F32)
            top = sb(ph, "top", [128, 8, 8], F32)
            selm = sb(ph, "selm", [128, 8, 16], F32)
            stT = [sb(ph, f"stT{i}", [128, 512], BF16) for i in range(2)]
            nq = 0
            for g in range(NG):
                sl = slice(g * 512, (g + 1) * 512)
                for pair in range(4):
                    qi = nq % 4
                    nq += 1
                    for hh in range(2):
                        P.op(SP, lambda qi=qi, hh=hh, pair=pair, sl=sl: sp.dma_start(out=qp[qi][:, hh, :],
                                                                                      in_=qm_s[2 * pair + hh, 0:64, sl]),
                             w=[f'qp{qi}'], dma=f'qp{qi}', waw=False)
                    for i in range(4):
                        for hh in range(2):
                            hg = 2 * pair + hh
                            P.op(T, lambda qi=qi, hh=hh, i=i, hg=hg, pair=pair: pe.matmul(
                                ps[0][:, i * 128 + hg * 16:i * 128 + hg * 16 + 16],
                                lhsT=qp[qi][:, hh, i * 128:(i + 1) * 128],
                                rhs=(ksb[0:64, pair, :] if hh == 0 else ksbB[:, pair, :]), start=True, stop=True),
                                 r=[f'qp{qi}', 'ksb', 'ksbB'], w=['ps0'], waw=(pair == 0 and i == 0 and hh == 0))
                P.op(V, lambda: ve.tensor_copy(out=gsb[:].rearrange("p a h n -> p (a h n)"), in_=ps[0][:]), r=['ps0'], w=['gsb'])
                P.op(V, lambda: ve.memset(sbt[:], NEG), r=[], w=['sbt'])
                for i in range(4):
                    tt = 4 * g + i
                    nb = tt // 2
                    if nb <= 3:
                        P.op(V, lambda i=i, nb=nb: ve.memset(sbt[:, i, :, 0:nb + 1], 0.0), w=['sbt'])
                        continue
                    P.op(V, lambda: ve.memset(tmp[:], -1e30), w=['tmp'])
                    P.op(V, lambda i=i, nb=nb: ve.tensor_copy(out=tmp[:, :, 0:nb], in_=gsb[:, i, :, 0:nb]), r=['gsb'], w=['tmp'])
                    for h in range(NH):
                        P.op(V, lambda h=h: ve.max(out=top[:, h, :], in_=tmp[:, h, :]), r=['tmp'], w=['top'], waw=(h == 0))
                    P.op(V, lambda: ve.tensor_tensor(out=selm[:], in0=tmp[:], in1=top[:, :, 2:3].to_broadcast([128, 8, 16]), op=ALU.is_ge),
                         r=['tmp', 'top'], w=['selm'])
                    P.op(V, lambda i=i: ve.tensor_scalar(out=sbt[:, i, :, :], in0=selm[:], scalar1=-NEG, scalar2=NEG,
                                                         op0=ALU.mult, op1=ALU.add), r=['selm'], w=['sbt'])
                    P.op(V, lambda i=i, nb=nb: ve.memset(sbt[:, i, :, nb:nb + 1], 0.0), w=['sbt'])
                P.op(V, lambda: ve.tensor_copy(out=sbb[:].rearrange("p a c -> p (a c)"), in_=sbt[:].rearrange("p a h n -> p (a h n)")),
                     r=['sbt'], w=['sbb'])
                pb1 = ps[1][:].bitcast(BF16)
                for i in range(4):
                    P.op(T, lambda i=i, pb1=pb1: pe.transpose(out=pb1[:, i * 128:(i + 1) * 128], in_=sbb[:, i, :], identity=ident_b[:]),
                         r=['sbb', 'ident_b'], w=['ps1'], waw=(i == 0))
                si = g % 2
                P.op(A, lambda si=si, pb1=pb1: ac.activation(out=stT[si][:], in_=pb1[:, 0:512], func=AF.Copy), r=['ps1'], w=[f'stT{si}'])
                for h in range(NH):
                    P.op(SP, lambda h=h, si=si, sl=sl: sp.dma_start(out=qm_s[h, 64:80, sl], in_=stT[si][h * 16:(h + 1) * 16, :]),
                         r=[f'stT{si}'], w=[f'qmaug{h}_{g}'], dma=f'stT{si}_st')
            P.flush(barrier=True)
        chk('A3')

        with ExitStack() as ph:
            qh = [sb(ph, f"qh{i}", [KM, S], BF16) for i in range(2)]
            kh = [sb(ph, f"kh{i}", [KM, S], BF16) for i in range(2)]
            vh = [sb(ph, f"vh{i}", [128, NT, 65], BF16) for i in range(2)]
            ccl = sb(ph, "ccl", [128, NT * 8], F32)
            pT = [sb(ph, f"pT{i}", [128, 512], BF16) for i in range(4)]
            osb = [sb(ph, f"osb{i}", [65, 512], F32) for i in range(2)]
            ysb = [sb(ph, f"ysb{i}", [64, 512], BF16) for i in range(2)]
            sel65 = sb(ph, "sel65", [65, 128], F32)
            P.op(V, lambda: ve.memset(sel65[:], 0.0), w=['sel65'])
            P.op(V, lambda: ve.memset(sel65[64:65, :], 1.0), w=['sel65'])
            P.op(SP, lambda: sp.dma_start(out=ccl[:], in_=ccol_s[:, :]), w=['ccl'], dma='ccl')
            cfgs = ((qf_s, kf_s, vf_s, yf_s, KF), (qm_s, km_s, vm_s, ym_s, KM))
            heads = [(br, h) for br in range(2) for h in range(NH)]
            if stop == 'B0':
                heads = heads[:1]
            elif stop == 'B':
                heads = heads[:NH]

            def emit_loads(idx):
                br, h = heads[idx]
                q_s, k_s, v_s, y_s, KK = cfgs[br]
                hb_ = idx % 2
                P.op(SP, lambda: sp.dma_start(out=qh[hb_][0:KK, :], in_=q_s[h, :, :]), w=[f'qh{hb_}'], dma=f'qh{hb_}')
                P.op(SP, lambda: sp.dma_start(out=kh[hb_][0:KK, :], in_=k_s[h, :, :]), w=[f'kh{hb_}'], dma=f'kh{hb_}')
                P.op(SP, lambda: sp.dma_start(out=vh[hb_][:], in_=v_s[h, :, :, :]), w=[f'vh{hb_}'], dma=f'vh{hb_}')

            tiles = []
            for idx, (br, h) in enumerate(heads):
                for g in range(NG):
                    for J in range(4 * g + 4):
                        tiles.append((idx, br, h, g, J))
            LA = 3
            deferred = []

            def emit_score(n):
                idx, br, h, g, J = tiles[n]
                KK = cfgs[br][4]
                hb_ = idx % 2
                pi = n % 4
                diag = J >= 4 * g
                c0 = 128 * (J - 4 * g) if diag else 0
                P.op(T, lambda: pe.matmul(ps[pi][:, c0:512], lhsT=kh[hb_][0:KK, J * 128:(J + 1) * 128], rhs=qh[hb_][0:KK, g * 512 + c0:(g + 1) * 512],
                                          start=True, stop=(not diag)), r=[f'kh{hb_}', f'qh{hb_}'], w=[f'ps{pi}'])
                if diag:
                    P.op(T, lambda: pe.matmul(ps[pi][:, c0:512], lhsT=ident_b[:], rhs=cmask[:, J - 4 * g, c0:512], start=False, stop=True),
                         r=['ident_b', 'cmask'], w=[f'ps{pi}'], waw=False)
                if br == 0:
                    P.op(A, lambda: ac.activation(out=pT[pi][:, c0:512], in_=ps[pi][:, c0:512], func=AF.Exp, bias=ccl[:, J * 8 + h:J * 8 + h + 1]),
                         r=[f'ps{pi}', 'ccl'], w=[f'pT{pi}'])
                else:
                    P.op(A, lambda: ac.activation(out=pT[pi][:, c0:512], in_=ps[pi][:, c0:512], func=AF.Exp, scale=0.125), r=[f'ps{pi}'], w=[f'pT{pi}'])

            def emit_pv(n):
                idx, br, h, g, J = tiles[n]
                y_s = cfgs[br][3]
                hb_ = idx % 2
                pi = n % 4
                gi = idx * NG + g
                po = 4 + gi % 2
                ob = gi % 2
                nj = 4 * g + 4
                c0 = 128 * (J - 4 * g) if J >= 4 * g else 0
                P.op(T, lambda: pe.matmul(ps[po][0:65, c0:512], lhsT=vh[hb_][:, J, :], rhs=pT[pi][:, c0:512], start=(J == 0), stop=(J == nj - 1)),
                     r=[f'vh{hb_}', f'pT{pi}'], w=[f'ps{po}'], waw=(J == 0))
                if J == nj - 1:
                    P.op(A, lambda: ac.activation(out=osb[ob][:], in_=ps[po][0:65, :], func=AF.Copy), r=[f'ps{po}'], w=[f'osb{ob}'])
                    P.op(V, lambda: ve.reciprocal(out=osb[ob][64:65, :], in_=osb[ob][64:65, :]), r=[f'osb{ob}'], w=[f'osb{ob}'])
                    pbc = 6 + ob

                    def stage2():
                        P.op(T, lambda: pe.matmul(ps[pbc][:], lhsT=sel65[:], rhs=osb[ob][:], start=True, stop=True),
                             r=['sel65', f'osb{ob}'], w=[f'ps{pbc}'])
                        P.op(V, lambda: ve.tensor_tensor(out=ysb[ob][:], in0=osb[ob][0:64, :], in1=ps[pbc][0:64, :], op=ALU.mult),
                             r=[f'osb{ob}', f'ps{pbc}'], w=[f'ysb{ob}'])
                        P.op(SP, lambda: sp.dma_start(out=y_s[h * 64:(h + 1) * 64, g * 512:(g + 1) * 512], in_=ysb[ob][:]),
                             r=[f'ysb{ob}'], w=[f'{y_s.name}_{h}_{g}'], dma=f'ysb{ob}_st')
                    deferred.append([n + 3, stage2])
                if g == 0 and J == 0 and idx + 1 < len(heads):
                    emit_loads(idx + 1)

            emit_loads(0)
            for n in range(len(tiles) + LA):
                if n < len(tiles):
                    emit_score(n)
                if n >= LA:
                    emit_pv(n - LA)
                    while deferred and deferred[0][0] <= n - LA:
                        deferred.pop(0)[1]()
            while deferred:
                deferred.pop(0)[1]()
            P.flush(barrier=True)
        chk('C')

        wgt = sb(es, "wgt", [128, NT, 2], F32)
        oh = sb(es, "oh", [128, 2, NT, NE], F32)
        pref = sb(es, "pref", [128, NT, NE], F32)
        run = sb(es, "run", [128, NE], F32)
        dI = sb(es, "dI", [128, 2, NT], I32)
        widx = sb(es, "widx", [128, NSLOT], I32)

        ixt = [sb(es, f"ixt{i}", [128, 1], I32) for i in range(6)]
        nix = [0]

        def idx_tile(src_ap, srcres):
            i = nix[0] % 6
            nix[0] += 1
            P.op(G, lambda: gp.tensor_copy(out=ixt[i][:], in_=src_ap), r=[srcres], w=[f'ixt{i}'])
            return ixt[i], f'ixt{i}'

        def rms_rstd(src, srcname, sqt, sst, rst, nm):
            P.op(A, lambda: ac.activation(out=sqt[:], in_=src, func=AF.Square, accum_out=sst[:]), r=[srcname], w=[nm + 'sq', nm + 'ss'])
            P.op(A, lambda: ac.activation(out=rst[:], in_=sst[:], func=AF.Ln, scale=1.0 / D, bias=EPS), r=[nm + 'ss'], w=[nm + 'rs'])
            P.op(A, lambda: ac.activation(out=rst[:], in_=rst[:], func=AF.Exp, scale=-0.5), r=[nm + 'rs'], w=[nm + 'rs'])

        with ExitStack() as ph:
            wfb = sb(ph, "wfb", [128, 4, D], BF16)
            wmb = sb(ph, "wmb", [128, 4, D], BF16)
            wo = sb(ph, "wo", [128, 8, D], BF16)
            wr = sb(ph, "wr", [128, 8, 36], F32)
            brt = sb(ph, "brt", [128, 36], F32)
            g2 = sb(ph, "g2", [128, D], F32)
            ustr = sb(ph, "ustr", [128, 128], BF16)
            onesm = sb(ph, "onesm", [128, 128], BF16)
            P.op(G, lambda: gp.dma_start(out=wfb[:], in_=wfb_d[:, :, :]), w=['wfb'], dma='wfb')
            P.op(G, lambda: gp.dma_start(out=wmb[:], in_=wmb_d[:, :, :]), w=['wmb'], dma='wmb')
            P.op(G, lambda: gp.dma_start(out=wo[:], in_=wo_d[:, :, :]), w=['wo'], dma='wo')
            P.op(G, lambda: gp.dma_start(out=ustr[:], in_=ustr_d[:, :]), w=['ustr'], dma='ustr')
            P.op(SP, lambda: sp.dma_start(out=wr[:], in_=wr_d[:, :, :]), w=['wr'], dma='wr')
            P.op(SP, lambda: sp.dma_start(out=brt[:], in_=br_d[:, :].partition_broadcast(128)), w=['brt'], dma='brt')
            P.op(SP, lambda: sp.dma_start(out=g2[:], in_=g2_d[:, :].partition_broadcast(128)), w=['g2'], dma='g2')
            P.op(V, lambda: ve.memset(onesm[:], 1.0), w=['onesm'])
            P.op(V, lambda: ve.memset(run[:], 0.0), w=['run'])
            yfT = [sb(ph, f"yfT{i}", [128, 4, 512], BF16) for i in range(2)]
            ymT = [sb(ph, f"ymT{i}", [128, 4, 512], BF16) for i in range(2)]
            sga = [sb(ph, f"sga{i}", [128, 8, 512], BF16) for i in range(2)]
            sgb = [sb(ph, f"sgb{i}", [128, 8, 512], BF16) for i in range(2)]
            mixT = sb(ph, "mixT", [128, 8, 512], BF16)
            ta = [sb(ph, f"ta{i}", [128, 512], F32) for i in range(2)]
            tb = [sb(ph, f"tb{i}", [128, 512], F32) for i in range(2)]
            xt = [sb(ph, f"dxt{i}", [128, D], F32) for i in range(2)]
            x1 = [sb(ph, f"x1_{i}", [128, D], F32) for i in range(2)]
            h2f = [sb(ph, f"h2f{i}", [128, D], F32) for i in range(2)]
            h2b = [sb(ph, f"h2b{i}", [128, D], BF16) for i in range(2)]
            h2T = [sb(ph, f"h2T{i}", [128, 8, 128], F32) for i in range(2)]
            sqd = sb(ph, "sqd", [128, D], BF16)
            ssd = [sb(ph, f"ssd{i}", [128, 1], F32) for i in range(2)]
            rsd = [sb(ph, f"rsd{i}", [128, 1], F32) for i in range(2)]
            lg = sb(ph, "lg", [128, 36], F32)
            sm = sb(ph, "sm", [128, 16], F32)
            gmask = sb(ph, "gmask", [128, 4], F32)
            flm = sb(ph, "flm", [128, 4, 8], F32)
            top8 = sb(ph, "top8", [128, 8], F32)
            gex = sb(ph, "gex", [128, 4], F32)
            mt = sb(ph, "mt", [128, NE], BF16)
            npd = 0
            d_pending = []

            def d_counts(tt):
                P.op(T, lambda: pe.matmul(ps[7][:, 0:32], lhsT=ustr[:], rhs=mt[:], start=True, stop=True), r=['ustr', 'mt'], w=['ps7'])
                P.op(T, lambda: pe.matmul(ps[7][:, 32:64], lhsT=onesm[:], rhs=mt[:], start=True, stop=True), r=['onesm', 'mt'], w=['ps7'], waw=False)
                P.op(V, lambda: ve.tensor_tensor(out=pref[:, tt, :], in0=ps[7][:, 0:32], in1=run[:], op=ALU.add), r=['ps7', 'run'], w=[f'pref{tt}'])
                P.op(V, lambda: ve.tensor_tensor(out=run[:], in0=ps[7][:, 32:64], in1=run[:], op=ALU.add), r=['ps7', 'run'], w=['run'])

            for g in range(NG):
                gb_ = g % 2
                sl = slice(g * 512, (g + 1) * 512)
                P.op(SP, lambda gb_=gb_, sl=sl: sp.dma_start(out=yfT[gb_][:], in_=yf_s[:, sl].rearrange("(k p) s -> p k s", p=128)), w=[f'yfT{gb_}'], dma=f'yfT{gb_}')
                P.op(SP, lambda gb_=gb_, sl=sl: sp.dma_start(out=ymT[gb_][:], in_=ym_s[:, sl].rearrange("(k p) s -> p k s", p=128)), w=[f'ymT{gb_}'], dma=f'ymT{gb_}')
                P.op(SP, lambda gb_=gb_, sl=sl: sp.dma_start(out=sga[gb_][:], in_=sga_s[:, :, sl].rearrange("c p s -> p c s")), w=[f'sga{gb_}'], dma=f'sga{gb_}')
                P.op(SP, lambda gb_=gb_, sl=sl: sp.dma_start(out=sgb[gb_][:], in_=sgb_s[:, :, sl].rearrange("c p s -> p c s")), w=[f'sgb{gb_}'], dma=f'sgb{gb_}')
                for c in range(8):
                    pa = (npd % 2) * 2
                    pb_ = pa + 1
                    ti = npd % 2
                    npd += 1
                    for kc in range(4):
                        P.op(T, lambda kc=kc, c=c, pa=pa, gb_=gb_: pe.matmul(ps[pa][:], lhsT=wfb[:, kc, c * 128:(c + 1) * 128], rhs=yfT[gb_][:, kc, :],
                                                                           start=(kc == 0), stop=(kc == 3)), r=['wfb', f'yfT{gb_}'], w=[f'ps{pa}'], waw=(kc == 0))
                    for kc in range(4):
                        P.op(T, lambda kc=kc, c=c, pb_=pb_, gb_=gb_: pe.matmul(ps[pb_][:], lhsT=wmb[:, kc, c * 128:(c + 1) * 128], rhs=ymT[gb_][:, kc, :],
                                                                             start=(kc == 0), stop=(kc == 3)), r=['wmb', f'ymT{gb_}'], w=[f'ps{pb_}'], waw=(kc == 0))
                    P.op(V, lambda c=c, pa=pa, ti=ti, gb_=gb_: ve.tensor_tensor(out=ta[ti][:], in0=sga[gb_][:, c, :], in1=ps[pa][:], op=ALU.mult),
                         r=[f'sga{gb_}', f'ps{pa}'], w=[f'ta{ti}'])
                    P.op(V, lambda c=c, pb_=pb_, ti=ti, gb_=gb_: ve.tensor_tensor(out=tb[ti][:], in0=sgb[gb_][:, c, :], in1=ps[pb_][:], op=ALU.mult),
                         r=[f'sgb{gb_}', f'ps{pb_}'], w=[f'tb{ti}'])
                    P.op(G, lambda c=c, ti=ti: gp.tensor_tensor(out=mixT[:, c, :], in0=ta[ti][:], in1=tb[ti][:], op=ALU.add),
                         r=[f'ta{ti}', f'tb{ti}'], w=[f'mixT{c}'])
                for i in range(4):
                    tt = 4 * g + i
                    b = tt % 2
                    P.op(SP, lambda tt=tt, b=b: sp.dma_start(out=xt[b][:], in_=x_d[tt * 128:(tt + 1) * 128, :]), w=[f'dxt{b}'], dma=f'dxt{b}')
                    for half in range(2):
                        po = 4 + half
                        for c in range(8):
                            P.op(T, lambda c=c, i=i, half=half, po=po: pe.matmul(ps[po][:], lhsT=mixT[:, c, i * 128:(i + 1) * 128],
                                                                                 rhs=wo[:, c, half * 512:(half + 1) * 512], start=(c == 0), stop=(c == 7)),
                                 r=['wo', f'mixT{c}'], w=[f'ps{po}'], waw=(c == 0))
                        P.op(V, lambda b=b, half=half, po=po: ve.tensor_tensor(out=x1[b][:, half * 512:(half + 1) * 512],
                                                                               in0=xt[b][:, half * 512:(half + 1) * 512], in1=ps[po][:], op=ALU.add),
                             r=[f'dxt{b}', f'ps{po}'], w=[f'x1_{b}'], waw=False)
                    while d_pending:
                        d_counts(d_pending.pop(0))
                    P.op(SP, lambda tt=tt, b=b: sp.dma_start(out=x1_s[tt * 128:(tt + 1) * 128, :], in_=x1[b][:]), r=[f'x1_{b}'], w=[f'x1s{tt}'], dma=f'x1_{b}_st')
                    rms_rstd(x1[b][:], f'x1_{b}', sqd, ssd[b], rsd[b], f'd{b}')
                    P.op(V, lambda b=b: ve.scalar_tensor_tensor(out=h2f[b][:], in0=x1[b][:], scalar=rsd[b][:, 0:1], in1=g2[:], op0=ALU.mult, op1=ALU.mult),
                         r=[f'x1_{b}', f'd{b}rs', 'g2'], w=[f'h2f{b}'])
                    P.op(G, lambda b=b: gp.tensor_copy(out=h2b[b][:], in_=h2f[b][:]), r=[f'h2f{b}'], w=[f'h2b{b}'])
                    P.op(SP, lambda tt=tt, b=b: sp.dma_start(out=h2_s[tt * 128:(tt + 1) * 128, :], in_=h2b[b][:]), r=[f'h2b{b}'], w=[f'h2s{tt}'], dma=f'h2b{b}_st')
                    for kc in range(8):
                        pt_ = 6 + kc // 4
                        P.op(T, lambda kc=kc, b=b, pt_=pt_: pe.transpose(out=ps[pt_][:, (kc % 4) * 128:(kc % 4 + 1) * 128], in_=h2f[b][:, kc * 128:(kc + 1) * 128],
                                                                         identity=ident_f[:]), r=[f'h2f{b}', 'ident_f'], w=[f'ps{pt_}'], waw=(kc % 4 == 0))
                    P.op(A, lambda b=b: ac.activation(out=h2T[b][:, 0:4, :], in_=ps[6][:].rearrange("p (k c) -> p k c", k=4), func=AF.Copy), r=['ps6'], w=[f'h2T{b}'], waw=False)
                    P.op(V, lambda b=b: ve.tensor_copy(out=h2T[b][:, 4:8, :], in_=ps[7][:].rearrange("p (k c) -> p k c", k=4)), r=['ps7'], w=[f'h2T{b}'], waw=False)
                    pr = 6
                    for kc in range(8):
                        P.op(T, lambda kc=kc, b=b: pe.matmul(ps[pr][:, 0:36], lhsT=h2T[b][:, kc, :], rhs=wr[:, kc, :], start=(kc == 0), stop=(kc == 7)),
                             r=[f'h2T{b}', 'wr'], w=[f'ps{pr}'], waw=(kc == 0))
                    P.op(V, lambda: ve.tensor_tensor(out=lg[:], in0=ps[pr][:, 0:36], in1=brt[:], op=ALU.add), r=[f'ps{pr}', 'brt'], w=['lg'])
                    P.op(V, lambda: ve.tensor_reduce(out=sm[:, 0:1], in_=lg[:, 0:4], axis=AX.X, op=ALU.max), r=['lg'], w=['sm0'])
                    P.op(V, lambda: ve.tensor_scalar(out=sm[:, 1:2], in0=sm[:, 0:1], scalar1=-1.0, scalar2=None, op0=ALU.mult), r=['sm0'], w=['sm1'])
                    P.op(A, lambda: ac.activation(out=gex[:], in_=lg[:, 0:4], func=AF.Exp, bias=sm[:, 1:2], accum_out=sm[:, 2:3]), r=['lg', 'sm1'], w=['gex', 'sm2'])
                    P.op(V, lambda: ve.reciprocal(out=sm[:, 3:4], in_=sm[:, 2:3]), r=['sm2'], w=['sm3'])
                    P.op(V, lambda: ve.tensor_scalar(out=gmask[:], in0=lg[:, 0:4], scalar1=sm[:, 0:1], scalar2=None, op0=ALU.is_ge), r=['lg', 'sm0'], w=['gmask'])
                    P.op(V, lambda: ve.tensor_scalar(out=gmask[:], in0=gmask[:], scalar1=1e30, scalar2=-1e30, op0=ALU.mult, op1=ALU.add), r=['gmask'], w=['gmask'])
                    P.op(V, lambda: ve.tensor_tensor(out=flm[:], in0=lg[:, 4:36].rearrange("p (g e) -> p g e", g=4),
                                                     in1=gmask[:].unsqueeze(2).to_broadcast([128, 4, 8]), op=ALU.add), r=['lg', 'gmask'], w=['flm'])
                    P.op(V, lambda: ve.max(out=top8[:], in_=flm[:].rearrange("p g e -> p (g e)")), r=['flm'], w=['top8'])
                    for k in range(2):
                        P.op(V, lambda k=k, tt=tt: ve.tensor_scalar(out=oh[:, k, tt, :], in0=flm[:].rearrange("p g e -> p (g e)"), scalar1=top8[:, k:k + 1],
                                                                    scalar2=None, op0=ALU.is_equal), r=['flm', 'top8'], w=[f'oh{k}_{tt}'])
                    P.op(V, lambda: ve.tensor_scalar(out=sm[:, 4:5], in0=top8[:, 0:1], scalar1=-1.0, scalar2=None, op0=ALU.mult), r=['top8'], w=['sm4'])
                    P.op(A, lambda: ac.activation(out=sm[:, 5:6], in_=top8[:, 1:2], func=AF.Exp, bias=sm[:, 4:5]), r=['top8', 'sm4'], w=['sm5'])
                    P.op(V, lambda: ve.tensor_scalar(out=sm[:, 6:7], in0=sm[:, 5:6], scalar1=1.0, scalar2=None, op0=ALU.add), r=['sm5'], w=['sm6'])
                    P.op(V, lambda: ve.reciprocal(out=sm[:, 7:8], in_=sm[:, 6:7]), r=['sm6'], w=['sm7'])
                    P.op(V, lambda tt=tt: ve.tensor_tensor(out=wgt[:, tt, 0:1], in0=sm[:, 3:4], in1=sm[:, 7:8], op=ALU.mult), r=['sm3', 'sm7'], w=[f'wgt0_{tt}'])
                    P.op(V, lambda tt=tt: ve.tensor_tensor(out=wgt[:, tt, 1:2], in0=wgt[:, tt, 0:1], in1=sm[:, 5:6], op=ALU.mult), r=[f'wgt0_{tt}', 'sm5'], w=[f'wgt1_{tt}'])
                    P.op(V, lambda tt=tt: ve.tensor_tensor(out=mt[:], in0=oh[:, 0, tt, :], in1=oh[:, 1, tt, :], op=ALU.add), r=[f'oh0_{tt}', f'oh1_{tt}'], w=['mt'])
                    d_pending.append(tt)
            while d_pending:
                d_counts(d_pending.pop(0))
            P.flush(barrier=True)
        chk('D')

        with ExitStack() as ph:
            jt = sb(ph, "jt", [128, NSLOT], F32)
            pidt = sb(ph, "pidt", [128, 1], F32)
            cmp = sb(ph, "cmp", [128, NSLOT, NE], F32)
            nblk = sb(ph, "nblk", [128, NE], F32)
            pend = sb(ph, "pend", [128, NE], F32)
            pst = sb(ph, "pst", [128, NE], F32)
            ones32 = sb(ph, "ones32", [128, NE], F32)
            tA = sb(ph, "tA", [128, NT, NE], F32)
            tB = sb(ph, "tB", [128, NT, NE], F32)
            dF = sb(ph, "dF", [128, 2, NT], F32)
            ebf = sb(ph, "ebf", [128, NSLOT], F32)
            hrow = [sb(ph, f"hrow{i}", [128, D], BF16) for i in range(2)]
            P.op(SP, lambda: sp.dma_start(out=jt[:], in_=jthr_d[:, :].partition_broadcast(128)), w=['jt'], dma='jt')
            P.op(SP, lambda: sp.dma_start(out=pidt[:], in_=pid_d[:, :]), w=['pidt'], dma='pidt')
            P.op(V, lambda: ve.memset(ones32[:], 1.0), w=['ones32'])
            P.op(V, lambda: ve.tensor_tensor(out=cmp[:, 0:NJ, :].rearrange("p j e -> p e j"), in0=run[:].unsqueeze(2).to_broadcast([128, NE, NJ]),
                                             in1=jt[:, 0:NJ].unsqueeze(1).to_broadcast([128, NE, NJ]), op=ALU.is_gt), r=['run', 'jt'], w=['cmp'])
            P.op(V, lambda: ve.tensor_reduce(out=nblk[:], in_=cmp[:, 0:NJ, :].rearrange("p j e -> p e j"), axis=AX.X, op=ALU.add), r=['cmp'], w=['nblk'])
            P.op(V, lambda: ve.tensor_scalar(out=nblk[:], in0=nblk[:], scalar1=float(SLOT), scalar2=None, op0=ALU.mult), r=['nblk'], w=['nblk'])
            P.op(V, lambda: ve.tensor_tensor_scan(out=pend[:], data0=ones32[:], data1=nblk[:], initial=0.0, op0=ALU.mult, op1=ALU.add),
                 r=['nblk', 'ones32'], w=['pend'])
            P.op(V, lambda: ve.tensor_tensor(out=pst[:], in0=pend[:], in1=nblk[:], op=ALU.subtract), r=['pend', 'nblk'], w=['pst'])
            allpref = [f'pref{t}' for t in range(NT)]
            P.op(V, lambda: ve.tensor_tensor(out=tA[:], in0=pref[:], in1=pst[:].unsqueeze(1).to_broadcast([128, NT, NE]), op=ALU.add),
                 r=allpref + ['pst'], w=['tA'])
            for k in range(2):
                P.op(V, lambda k=k: ve.tensor_tensor(out=tB[:], in0=tA[:], in1=oh[:, k, :, :], op=ALU.mult), r=['tA'] + [f'oh{k}_{t}' for t in range(NT)], w=['tB'])
                P.op(V, lambda k=k: ve.tensor_reduce(out=dF[:, k, :], in_=tB[:], axis=AX.X, op=ALU.add), r=['tB'], w=[f'dF{k}'])
            P.op(V, lambda: ve.tensor_copy(out=dI[:], in_=dF[:]), r=['dF0', 'dF1'], w=['dI'])
            P.op(V, lambda: ve.tensor_tensor(out=cmp[:], in0=pend[:].unsqueeze(1).to_broadcast([128, NSLOT, NE]),
                                             in1=jt[:].unsqueeze(2).to_broadcast([128, NSLOT, NE]), op=ALU.is_le), r=['pend', 'jt'], w=['cmp'])
            P.op(V, lambda: ve.tensor_reduce(out=ebf[:], in_=cmp[:], axis=AX.X, op=ALU.add), r=['cmp'], w=['ebf'])
            P.op(V, lambda: ve.tensor_scalar(out=ebf[:], in0=ebf[:], scalar1=float(NE), scalar2=128.0, op0=ALU.min, op1=ALU.mult), r=['ebf'], w=['ebf'])
            P.op(V, lambda: ve.tensor_scalar(out=ebf[:], in0=ebf[:], scalar1=pidt[:, 0:1], scalar2=None, op0=ALU.add), r=['ebf', 'pidt'], w=['ebf'])
            P.op(V, lambda: ve.tensor_copy(out=widx[:], in_=ebf[:]), r=['ebf'], w=['widx'])
            if 'dbgI_s' in dbg:
                P.op(SP, lambda: sp.dma_start(out=dbgI_s[:, 0:2 * NT], in_=dI[:].rearrange("p k t -> p (k t)")), r=['dI'], w=['dbgI_a'], dma='dbgI')
                P.op(SP, lambda: sp.dma_start(out=dbgI_s[:, 2 * NT:], in_=widx[:]), r=['widx'], w=['dbgI_b'], dma='dbgI')
            chk('E1a')
            for tt in range(NT):
                b = tt % 2
                P.op(SP, lambda tt=tt, b=b: sp.dma_start(out=hrow[b][:], in_=h2_s[tt * 128:(tt + 1) * 128, :]), w=[f'hrow{b}'], dma=f'hrow{b}')
                for k in range(2):
                    it, itn = idx_tile(dI[:, k, tt:tt + 1], 'dI')
                    P.op(G, lambda tt=tt, b=b, k=k, it=it: gp.indirect_dma_start(out=xs_s[:, :], out_offset=bass.IndirectOffsetOnAxis(ap=it[:, :], axis=0),
                                                                                in_=hrow[b][:], in_offset=None),
                         r=[f'hrow{b}', itn], w=[f'xs_{tt}_{k}'], dma=f'hrow{b}_st')
            P.flush(barrier=True)
        chk('E1')

        with ExitStack() as ph:
            NB3 = 3
            wg = [sb(ph, f"wg{i}", [128, 8, DE], BF16) for i in range(NB3)]
            wu = [sb(ph, f"wu{i}", [128, 8, DE], BF16) for i in range(NB3)]
            wd = [sb(ph, f"wd{i}", [128, 4, D], BF16) for i in range(NB3)]
            rows = [sb(ph, f"rows{i}", [128, 4, D], BF16) for i in range(NB3)]
            rT = [sb(ph, f"rT{i}", [128, 8, SLOT], BF16) for i in range(NB3)]
            sg = [sb(ph, f"sg{i}", [128, SLOT], F32) for i in range(2)]
            aT = [sb(ph, f"aT{i}", [128, 4, SLOT], BF16) for i in range(2)]
            orow = [sb(ph, f"orow{i}", [128, 4, D], F32) for i in range(2)]
            ntr = 0
            ngu = 0
            nout = 0

            def slot_stage1(b_):
                nonlocal ntr
                s2 = b_ % NB3
                it, itn = idx_tile(widx[:, b_:b_ + 1], 'widx')
                for (wt, wdram, nm) in ((wg, wg_d, 'wg'), (wu, wu_d, 'wu'), (wd, wd_d, 'wd')):
                    P.op(G, lambda wt=wt, wdram=wdram: gp.indirect_dma_start(
                        out=wt[s2][:].rearrange("p a c -> p (a c)"), out_offset=None, in_=wdram[:, :],
                        in_offset=bass.IndirectOffsetOnAxis(ap=it[:, :], axis=0), bounds_check=bc_reg, oob_is_err=False),
                         r=[itn], w=[f'{nm}{s2}'], dma=f'{nm}{s2}')
                P.op(A, lambda: ac.dma_start(out=rows[s2][:], in_=xs_s[b_ * SLOT:(b_ + 1) * SLOT, :].rearrange("(a p) d -> p a d", p=128)),
                     w=[f'rows{s2}'], dma=f'rows{s2}')
                for a_ in range(4):
                    pt_ = ntr % 2
                    ntr += 1
                    pbt = ps[pt_][:].bitcast(BF16)
                    for kc in range(8):
                        P.op(T, lambda kc=kc, pbt=pbt, a_=a_: pe.transpose(out=pbt[:, kc * 128:(kc + 1) * 128], in_=rows[s2][:, a_, kc * 128:(kc + 1) * 128],
                                                                         identity=ident_b[:]), r=[f'rows{s2}', 'ident_b'], w=[f'ps{pt_}'], waw=(kc == 0))
                    if a_ % 2 == 0:
                        P.op(A, lambda pbt=pbt, a_=a_: ac.activation(out=rT[s2][:, :, a_ * 128:(a_ + 1) * 128], in_=pbt.rearrange("p (k c) -> p k c", k=8), func=AF.Copy),
                             r=[f'ps{pt_}'], w=[f'rT{s2}'], waw=False)
                    else:
                        P.op(V, lambda pbt=pbt, a_=a_: ve.tensor_copy(out=rT[s2][:, :, a_ * 128:(a_ + 1) * 128], in_=pbt.rearrange("p (k c) -> p k c", k=8)),
                             r=[f'ps{pt_}'], w=[f'rT{s2}'], waw=False)

            def slot_stage2(b_):
                nonlocal ngu, nout
                s2 = b_ % NB3
                so = b_ % 2
                for j in range(4):
                    pg_ = 2 + ngu % 2
                    pu_ = 4 + ngu % 2
                    ngu += 1
                    for (wt, pp_, nm) in ((wg, pg_, 'wg'), (wu, pu_, 'wu')):
                        for kc in range(8):
                            P.op(T, lambda wt=wt, pp_=pp_, kc=kc, j=j: pe.matmul(ps[pp_][:], lhsT=wt[s2][:, kc, j * 128:(j + 1) * 128], rhs=rT[s2][:, kc, :],
                                                                               start=(kc == 0), stop=(kc == 7)),
                                 r=[f'{nm}{s2}', f'rT{s2}'], w=[f'ps{pp_}'], waw=(kc == 0))
                    sgi = j % 2
                    P.op(A, lambda pg_=pg_, sgi=sgi: ac.activation(out=sg[sgi][:], in_=ps[pg_][:], func=AF.Silu), r=[f'ps{pg_}'], w=[f'sg{sgi}'])
                    P.op(V, lambda pu_=pu_, sgi=sgi, j=j: ve.tensor_tensor(out=aT[so][:, j, :], in0=sg[sgi][:], in1=ps[pu_][:], op=ALU.mult),
                         r=[f'sg{sgi}', f'ps{pu_}'], w=[f'aT{so}_{j}'])
                for a_ in range(4):
                    for half in range(2):
                        po = 6 + nout % 2
                        nout += 1
                        for j in range(4):
                            P.op(T, lambda half=half, po=po, j=j, a_=a_: pe.matmul(ps[po][:], lhsT=aT[so][:, j, a_ * 128:(a_ + 1) * 128],
                                                                                 rhs=wd[s2][:, j, half * 512:(half + 1) * 512], start=(j == 0), stop=(j == 3)),
                                 r=[f'aT{so}_{j}', f'wd{s2}'], w=[f'ps{po}'], waw=(j == 0))
                        if half == 0:
                            P.op(A, lambda po=po, a_=a_: ac.activation(out=orow[so][:, a_, 0:512], in_=ps[po][:], func=AF.Copy), r=[f'ps{po}'], w=[f'orow{so}'], waw=False)
                        else:
                            P.op(V, lambda po=po, a_=a_: ve.tensor_copy(out=orow[so][:, a_, 512:1024], in_=ps[po][:]), r=[f'ps{po}'], w=[f'orow{so}'], waw=False)
                P.op(SP, lambda: sp.dma_start(out=or_s[b_ * SLOT:(b_ + 1) * SLOT, :].rearrange("(a p) d -> p a d", p=128), in_=orow[so][:]),
                     r=[f'orow{so}'], w=[f'ors{b_}'], dma=f'orow{so}_st')

            slot_stage1(0)
            slot_stage1(1)
            for b_ in range(NSLOT):
                if b_ + 2 < NSLOT:
                    slot_stage1(b_ + 2)
                slot_stage2(b_)
            P.flush(barrier=True)
        chk('E2')

        with ExitStack() as ph:
            wpg = sb(ph, "wpg", [128, 8, D], BF16)
            wpp = sb(ph, "wpp", [128, 2, D], BF16)
            g3 = sb(ph, "g3", [128, D], F32)
            g4 = sb(ph, "g4", [128, D], F32)
            P.op(G, lambda: gp.dma_start(out=wpg[:], in_=wpg_d[:, :, :]), w=['wpg'], dma='wpg')
            P.op(G, lambda: gp.dma_start(out=wpp[:], in_=wpp_d[:, :, :]), w=['wpp'], dma='wpp')
            P.op(SP, lambda: sp.dma_start(out=g3[:], in_=g3_d[:, :].partition_broadcast(128)), w=['g3'], dma='g3')
            P.op(SP, lambda: sp.dma_start(out=g4[:], in_=g4_d[:, :].partition_broadcast(128)), w=['g4'], dma='g4')
            r1 = [sb(ph, f"r1_{i}", [128, D], F32) for i in range(2)]
            r2 = [sb(ph, f"r2_{i}", [128, D], F32) for i in range(2)]
            xa = [sb(ph, f"xa{i}", [128, D], F32) for i in range(3)]
            pt = [sb(ph, f"pt{i}", [128, PLE], F32) for i in range(2)]
            ptb = [sb(ph, f"ptb{i}", [128, PLE], BF16) for i in range(2)]
            pTt = [sb(ph, f"pTt{i}", [128, 2, 128], BF16) for i in range(2)]
            h3 = [sb(ph, f"h3_{i}", [128, D], BF16) for i in range(2)]
            h3T = [sb(ph, f"h3T{i}", [128, 8, 128], BF16) for i in range(2)]
            gs = [sb(ph, f"gs{i}", [128, D], F32) for i in range(2)]
            ot = [sb(ph, f"ot{i}", [128, D], F32) for i in range(2)]
            sqf = sb(ph, "sqf", [128, D], BF16)
            ssf = [sb(ph, f"ssf{i}", [128, 1], F32) for i in range(2)]
            rsf = [sb(ph, f"rsf{i}", [128, 1], F32) for i in range(2)]
            ssg = [sb(ph, f"ssg{i}", [128, 1], F32) for i in range(2)]
            rsg = [sb(ph, f"rsg{i}", [128, 1], F32) for i in range(2)]
            def f_stage1(tt):
                b = tt % 2
                b3 = tt % 3
                tsl = slice(tt * 128, (tt + 1) * 128)
                for k, rr in enumerate((r1, r2)):
                    it, itn = idx_tile(dI[:, k, tt:tt + 1], 'dI')
                    P.op(G, lambda b=b, it=it, rr=rr: gp.indirect_dma_start(out=rr[b][:], out_offset=None, in_=or_s[:, :],
                                                                            in_offset=bass.IndirectOffsetOnAxis(ap=it[:, :], axis=0)),
                         r=[itn], w=[f'r{k + 1}_{b}'], dma=f'r{k + 1}_{b}')
                P.op(SP, lambda tsl=tsl, b3=b3: sp.dma_start(out=xa[b3][:], in_=x1_s[tsl, :]), w=[f'xa{b3}'], dma=f'xa{b3}')
                P.op(SP, lambda tsl=tsl, b=b: sp.dma_start(out=pt[b][:], in_=p_d[tsl, :]), w=[f'pt{b}'], dma=f'pt{b}')
                P.op(V, lambda tt=tt, b=b, b3=b3: ve.scalar_tensor_tensor(out=xa[b3][:], in0=r1[b][:], scalar=wgt[:, tt, 0:1], in1=xa[b3][:], op0=ALU.mult, op1=ALU.add),
                     r=[f'r1_{b}', f'xa{b3}'], w=[f'xa{b3}'])
                P.op(V, lambda tt=tt, b=b, b3=b3: ve.scalar_tensor_tensor(out=xa[b3][:], in0=r2[b][:], scalar=wgt[:, tt, 1:2], in1=xa[b3][:], op0=ALU.mult, op1=ALU.add),
                     r=[f'r2_{b}', f'xa{b3}'], w=[f'xa{b3}'])
                rms_rstd(xa[b3][:], f'xa{b3}', sqf, ssf[b], rsf[b], f'f{b}')
                P.op(V, lambda b=b, b3=b3: ve.scalar_tensor_tensor(out=h3[b][:], in0=xa[b3][:], scalar=rsf[b][:, 0:1], in1=g3[:], op0=ALU.mult, op1=ALU.mult),
                     r=[f'xa{b3}', f'f{b}rs', 'g3'], w=[f'h3_{b}'])
                P.op(G, lambda b=b: gp.tensor_copy(out=ptb[b][:], in_=pt[b][:]), r=[f'pt{b}'], w=[f'ptb{b}'])

            def f_stageT(tt):
                b = tt % 2
                pbt = ps[b][:].bitcast(BF16)
                for kc in range(8):
                    P.op(T, lambda kc=kc, b=b, pbt=pbt: pe.transpose(out=pbt[:, kc * 128:(kc + 1) * 128], in_=h3[b][:, kc * 128:(kc + 1) * 128], identity=ident_b[:]),
                         r=[f'h3_{b}', 'ident_b'], w=[f'ps{b}'], waw=(kc == 0))
                P.op(A, lambda b=b, pbt=pbt: ac.activation(out=h3T[b][:], in_=pbt.rearrange("p (k c) -> p k c", k=8), func=AF.Copy), r=[f'ps{b}'], w=[f'h3T{b}'])
                pb2 = ps[2 + b][:].bitcast(BF16)
                for kc in range(2):
                    P.op(T, lambda kc=kc, b=b, pb2=pb2: pe.transpose(out=pb2[:, kc * 128:(kc + 1) * 128], in_=ptb[b][:, kc * 128:(kc + 1) * 128], identity=ident_b[:]),
                         r=[f'ptb{b}', 'ident_b'], w=[f'ps{2 + b}'], waw=(kc == 0))
                P.op(V, lambda b=b, pb2=pb2: ve.tensor_copy(out=pTt[b][:], in_=pb2[:, 0:256].rearrange("p (k c) -> p k c", k=2)), r=[f'ps{2 + b}'], w=[f'pTt{b}'])


            def f_stage2(tt):
                b = tt % 2
                b3 = tt % 3
                tsl = slice(tt * 128, (tt + 1) * 128)
                for half in range(2):
                    hs = slice(half * 512, (half + 1) * 512)
                    pgt = 4 + half
                    ppp = 6 + half
                    for kc in range(8):
                        P.op(T, lambda kc=kc, b=b, hs=hs, pgt=pgt: pe.matmul(ps[pgt][:], lhsT=h3T[b][:, kc, :], rhs=wpg[:, kc, hs], start=(kc == 0), stop=(kc == 7)),
                             r=[f'h3T{b}', 'wpg'], w=[f'ps{pgt}'], waw=(kc == 0))
                    P.op(A, lambda b=b, hs=hs, pgt=pgt: ac.activation(out=gs[b][:, hs], in_=ps[pgt][:], func=AF.Sigmoid), r=[f'ps{pgt}'], w=[f'gs{b}'], waw=False)
                    for kc in range(2):
                        P.op(T, lambda kc=kc, b=b, hs=hs, ppp=ppp: pe.matmul(ps[ppp][:], lhsT=pTt[b][:, kc, :], rhs=wpp[:, kc, hs], start=(kc == 0), stop=(kc == 1)),
                             r=[f'pTt{b}', 'wpp'], w=[f'ps{ppp}'], waw=(kc == 0))
                    P.op(V, lambda b=b, hs=hs, ppp=ppp: ve.tensor_tensor(out=gs[b][:, hs], in0=gs[b][:, hs], in1=ps[ppp][:], op=ALU.mult),
                         r=[f'gs{b}', f'ps{ppp}'], w=[f'gs{b}'])
                P.op(V, lambda b=b, b3=b3: ve.tensor_tensor(out=xa[b3][:], in0=xa[b3][:], in1=gs[b][:], op=ALU.add), r=[f'xa{b3}', f'gs{b}'], w=[f'xa{b3}'])
                rms_rstd(xa[b3][:], f'xa{b3}', sqf, ssg[b], rsg[b], f'g{b}')
                P.op(V, lambda b=b, b3=b3: ve.scalar_tensor_tensor(out=ot[b][:], in0=xa[b3][:], scalar=rsg[b][:, 0:1], in1=g4[:], op0=ALU.mult, op1=ALU.mult),
                     r=[f'xa{b3}', f'g{b}rs', 'g4'], w=[f'ot{b}'])
                P.op(SP, lambda tsl=tsl, b=b: sp.dma_start(out=out_d[tsl, :], in_=ot[b][:]), r=[f'ot{b}'], w=[f'out{tt}'], dma=f'ot{b}_st')

            f_stage1(0)
            f_stage1(1)
            f_stageT(0)
            for tt in range(NT):
                if tt + 2 < NT:
                    f_stage1(tt + 2)
                if tt + 1 < NT:
                    f_stageT(tt + 1)
                f_stage2(tt)
            P.flush(barrier=True)

        P.flush(barrier=True)
    except _Stop:
        pass
    return nc


def _cmask():
    s_ = np.arange(128)[:, None, None]
    j_ = np.arange(4)[None, :, None]
    t_ = np.arange(512)[None, None, :]
    return np.where(t_ - s_ - 128 * j_ >= 0, 0.0, NEG).astype(np.float32)


def _host_layout(inputs):
    w_in = np.asarray(inputs['w_in'])[0]
    cols = []
    qf = np.arange(0, 512); kf = np.arange(512, 1024); vf = np.arange(1024, 1536)
    qm = np.arange(1536, 2048); km = np.arange(2048, 2560); vm = np.arange(2560, 3072)
    fl = np.arange(3072, 3080); ga = np.arange(3080, 4104); gb = np.arange(4104, 5128)

    def swap(c):
        c = c.reshape(8, 2, 32)
        return c[:, ::-1, :].reshape(-1)

    fm = np.concatenate([qf, kf, qm, swap(qm), km, swap(km), ga, gb])
    w_fm = w_in[:, fm].reshape(8, 128, 40, 128).transpose(2, 1, 0, 3)
    w_tm = np.stack([w_in[:, vf], w_in[:, vm]]).reshape(2, 8, 128, 512).transpose(0, 2, 1, 3)
    w_fl = w_in[:, fl].reshape(8, 128, 8).transpose(1, 0, 2)
    inv = 1.0 / (10000.0 ** (np.arange(0, 64, 2, dtype=np.float32) / 64.0))
    ang = np.arange(S, dtype=np.float32)[None, :] * inv[:, None]
    cos, sin = np.cos(ang).astype(np.float32), np.sin(ang).astype(np.float32)
    cs1 = np.concatenate([cos, cos, cos, cos], 0)
    cs2 = np.concatenate([-sin, sin, -sin, sin], 0)
    shared = dict(
        w_fm=np.ascontiguousarray(w_fm, dtype=np.float32), w_tm=np.ascontiguousarray(w_tm, dtype=np.float32),
        w_fl=np.ascontiguousarray(w_fl, dtype=np.float32),
        attn_norm=np.asarray(inputs['attn_norm'], dtype=np.float32).reshape(1, D),
        b_forget=np.asarray(inputs['b_forget'], dtype=np.float32).reshape(8, 1),
        cs1=np.ascontiguousarray(cs1), cs2=np.ascontiguousarray(cs2),
        ident=np.eye(128, dtype=np.float32),
        w_fb=np.ascontiguousarray(np.asarray(inputs['w_fox_branch'], np.float32)[0].reshape(4, 128, D).transpose(1, 0, 2)),
        w_mb=np.ascontiguousarray(np.asarray(inputs['w_moba_branch'], np.float32)[0].reshape(4, 128, D).transpose(1, 0, 2)),
        w_o=np.ascontiguousarray(np.asarray(inputs['w_out'], np.float32)[0].reshape(8, 128, D).transpose(1, 0, 2)),
        moe_norm=np.asarray(inputs['moe_norm'], np.float32).reshape(1, D),
        w_r=np.ascontiguousarray(np.concatenate([np.asarray(inputs['w_group'], np.float32)[0], np.asarray(inputs['w_fine'], np.float32)[0]], 1)
                                 .reshape(8, 128, 36).transpose(1, 0, 2)),
        b_r=np.concatenate([np.asarray(inputs['b_group'], np.float32)[0], np.asarray(inputs['b_fine'], np.float32)[0]]).reshape(1, 36),
        ustrict=np.triu(np.ones((128, 128), np.float32), 1),
        jthr=(float(SLOT) * np.arange(NSLOT, dtype=np.float32)).reshape(1, NSLOT),
        pid=np.arange(128, dtype=np.float32).reshape(128, 1),
        w_g=np.ascontiguousarray(np.asarray(inputs['w_gate'], np.float32)[0].reshape(NE, 8, 128, DE).transpose(0, 2, 1, 3)).reshape(NE * 128, 8 * DE),
        w_u=np.ascontiguousarray(np.asarray(inputs['w_up'], np.float32)[0].reshape(NE, 8, 128, DE).transpose(0, 2, 1, 3)).reshape(NE * 128, 8 * DE),
        w_d=np.ascontiguousarray(np.asarray(inputs['w_down'], np.float32)[0].reshape(NE, 4, 128, D).transpose(0, 2, 1, 3)).reshape(NE * 128, 4 * D),
        ple_norm=np.asarray(inputs['ple_norm'], np.float32).reshape(1, D),
        final_norm=np.asarray(inputs['final_norm'], np.float32).reshape(1, D),
        w_pg=np.ascontiguousarray(np.asarray(inputs['w_ple_gate'], np.float32)[0].reshape(8, 128, D).transpose(1, 0, 2)),
        w_pp=np.ascontiguousarray(np.asarray(inputs['w_ple_proj'], np.float32)[0].reshape(2, 128, D).transpose(1, 0, 2)),
        cmask=_cmask(), blk1h=np.kron(np.eye(16, dtype=np.float32), np.ones((1, 256), np.float32)),
    )
    return shared


def kernel(**inputs):
    shared = _host_layout(inputs)
    x = np.asarray(inputs['x'], dtype=np.float32)
    p = np.asarray(inputs['p'], dtype=np.float32)[0]
    nc = build()
    in_maps = []
    for c in range(8):
        m = dict(shared)
        m['x'] = np.ascontiguousarray(x[c])
        m['p'] = np.ascontiguousarray(p[c])
        in_maps.append(m)
    res = run_bass_kernel_spmd(nc, in_maps, core_ids=list(range(8)))
    return np.stack([r['out'] for r in res.results], 0)
```

```python
import numpy as np
from contextlib import ExitStack
import concourse.bass as bass
import concourse.mybir as mybir
from concourse.bass_utils import run_bass_kernel_spmd
import ml_dtypes

F32 = mybir.dt.float32
BF16 = mybir.dt.bfloat16
I32 = mybir.dt.int32
AF = mybir.ActivationFunctionType
ALU = mybir.AluOpType
AX = mybir.AxisListType

S = 4096
D = 1024
NT = S // 128
NG = S // 512
NH = 8
HD = 64
KF = 67
KM = 80
NEG = -30000.0
NE = 32
DE = 512
NSLOT = 48
SLOT = 512
NJ = S // SLOT
PLE = 256
EPS = 1e-6


class Prog:
    def __init__(self, nc, es):
        self.nc, self.es = nc, es
        self.eng = {'pe': nc.tensor, 'act': nc.scalar, 'dve': nc.vector, 'pool': nc.gpsimd, 'sp': nc.sync}
        self.ops = []
        self.emitted = 0
        self.wr = {}
        self.rd = {}
        self.prd = {}
        self.cur = {}
        self.dsem = {}
        self.done = {}
        self.waited = {}
        self.nsem = 0
        self.allsems = []
        self.freed = {'sp': [], 'pool': [], 'act': []}
        self.dsem_eng = {}

    def newsem(self, name):
        self.nsem += 1
        s = self.es.enter_context(self.nc.semaphore(f"{name}_{self.nsem}"))
        return s

    def op(self, eng, fn, r=(), w=(), dma=None, waw=True):
        i = len(self.ops)
        deps = set()
        for x in r:
            deps.update(self.wr.get(x, ()))
            self.rd.setdefault(x, []).append(i)
        for x in w:
            rds = self.rd.get(x, [])
            if rds:
                deps.update(rds)
                deps.update(self.wr.get(x, ()))
                self.wr[x] = [i]
                self.prd[x] = rds
                self.rd[x] = []
            elif waw:
                deps.update(self.wr.get(x, ()))
                deps.update(self.prd.get(x, ()))
                self.wr[x] = [i]
            else:
                deps.update(self.prd.get(x, ()))
                self.wr.setdefault(x, []).append(i)
        deps.discard(i)
        self.ops.append([eng, fn, deps, dma])
        return i

    def _skip(self, d, eng):
        deng, _, _, ddma = self.ops[d]
        return ddma is None and deng == 'pe' and eng == 'pe'

    def flush(self, barrier=True):
        n = len(self.ops)
        needed = set()
        for i in range(self.emitted, n):
            eng, fn, deps, dma = self.ops[i]
            for d in deps:
                if not self._skip(d, eng):
                    needed.add(d)
        last = {}
        for i in range(self.emitted, n):
            if self.ops[i][3] is None:
                last[self.ops[i][0]] = i
        needed.update(last.values())
        for lst in list(self.wr.values()) + list(self.rd.values()) + list(self.prd.values()):
            needed.update(lst)
        for i in range(self.emitted, n):
            eng, fn, deps, dma = self.ops[i]
            E = self.eng[eng]
            need = {}
            for d in deps:
                if self._skip(d, eng):
                    continue
                sem, val, dent = self.done[d]
                if dent is not None:
                    val = dent[1]
                k = id(sem)
                if need.get(k, (None, 0))[1] < val:
                    need[k] = (sem, val)
            for k, (sem, val) in need.items():
                if self.waited.get((eng, k), 0) >= val:
                    continue
                E.wait_ge(sem, val)
                self.waited[(eng, k)] = val
            ins = fn()
            if dma is not None:
                ent = self.dsem.get(dma)
                if ent is None:
                    self.dsem_eng[dma] = eng
                    if self.freed[eng]:
                        ent = self.dsem[dma] = self.freed[eng].pop()
                    else:
                        ent = self.dsem[dma] = [self.newsem('d'), 0]
                        self.allsems.append(ent)
                ent[1] += 16
                ins.then_inc(ent[0], 16)
                self.done[i] = (ent[0], ent[1], ent)
            elif i in needed:
                ent = self.cur.get(eng)
                if ent is None or ent[1] >= 6000:
                    ent = self.cur[eng] = [self.newsem('e' + eng), 0]
                    self.allsems.append(ent)
                ent[1] += 1
                ins.then_inc(ent[0], 1)
                self.done[i] = (ent[0], ent[1], None)
            self.ops[i][1] = None
        self.emitted = n
        if barrier:
            for eng, E in self.eng.items():
                for ent in self.allsems:
                    if ent[1] == 0:
                        continue
                    k = id(ent[0])
                    if self.waited.get((eng, k), 0) >= ent[1]:
                        continue
                    E.wait_ge(ent[0], ent[1])
                    self.waited[(eng, k)] = ent[1]
            self.wr = {}
            self.rd = {}
            self.prd = {}
            for k_, ent in self.dsem.items():
                self.freed[self.dsem_eng[k_]].append(ent)
            self.dsem = {}


def build(debug=(), stop=None):
    nc = bass.Bass("TRN2", target_bir_lowering=False)
    dbg = set(debug)

    def dram_in(name, shape, dt=F32):
        return nc.dram_tensor(name, list(shape), dt, kind="ExternalInput")

    def scratch(name, shape, dt):
        return nc.dram_tensor(name, list(shape), dt, kind=("ExternalOutput" if name in dbg else "Internal"))

    x_d = dram_in("x", [S, D])
    p_d = dram_in("p", [S, PLE])
    wfm_d = dram_in("w_fm", [40, 128, 8, 128])
    wtm_d = dram_in("w_tm", [2, 128, 8, 512])
    wfl_d = dram_in("w_fl", [128, 8, 8])
    g1_d = dram_in("attn_norm", [1, D])
    bf_d = dram_in("b_forget", [8, 1])
    cs1_d = dram_in("cs1", [128, S])
    cs2_d = dram_in("cs2", [128, S])
    id_d = dram_in("ident", [128, 128])
    out_d = nc.dram_tensor("out", [S, D], F32, kind="ExternalOutput")

    qf_s = scratch("qf_s", [NH, KF, S], BF16)
    kf_s = scratch("kf_s", [NH, KF, S], BF16)
    vf_s = scratch("vf_s", [NH, 128, NT, 65], BF16)
    qm_s = scratch("qm_s", [NH, KM, S], BF16)
    km_s = scratch("km_s", [NH, KM, S], BF16)
    vm_s = scratch("vm_s", [NH, 128, NT, 65], BF16)
    sga_s = scratch("sga_s", [8, 128, S], BF16)
    sgb_s = scratch("sgb_s", [8, 128, S], BF16)
    cneg_s = scratch("cneg_s", [8, S], F32)
    ccol_s = scratch("ccol_s", [128, NT * 8], F32)
    yf_s = scratch("yf_s", [512, S], BF16)
    ym_s = scratch("ym_s", [512, S], BF16)
    cmask_d = dram_in("cmask", [128, 4, 512])
    wfb_d = dram_in("w_fb", [128, 4, D])
    wmb_d = dram_in("w_mb", [128, 4, D])
    wo_d = dram_in("w_o", [128, 8, D])
    g2_d = dram_in("moe_norm", [1, D])
    wr_d = dram_in("w_r", [128, 8, 36])
    br_d = dram_in("b_r", [1, 36])
    ustr_d = dram_in("ustrict", [128, 128])
    jthr_d = dram_in("jthr", [1, NSLOT])
    pid_d = dram_in("pid", [128, 1])
    if stop in (None, 'E2'):
        wg_d = dram_in("w_g", [NE * 128, 8 * DE])
        wu_d = dram_in("w_u", [NE * 128, 8 * DE])
        wd_d = dram_in("w_d", [NE * 128, 4 * D])
    g3_d = dram_in("ple_norm", [1, D])
    g4_d = dram_in("final_norm", [1, D])
    wpg_d = dram_in("w_pg", [128, 8, D])
    wpp_d = dram_in("w_pp", [128, 2, D])
    x1_s = scratch("x1_s", [S, D], F32)
    h2_s = scratch("h2_s", [S, D], BF16)
    xs_s = scratch("xs_s", [NSLOT * SLOT, D], BF16)
    or_s = scratch("or_s", [NSLOT * SLOT, D], F32)
    dbgI_s = scratch("dbgI_s", [128, 2 * NT + NSLOT], I32)
    blk1h_d = dram_in("blk1h", [16, S])

    class _Stop(Exception):
        pass

    try:
      with ExitStack() as es:
        P = Prog(nc, es)

        def chk(tag):
            if stop == tag:
                P.flush(barrier=True)
                raise _Stop()
        V, A, T, G, SP = 'dve', 'act', 'pe', 'pool', 'sp'
        ve, ac, pe, gp, sp = nc.vector, nc.scalar, nc.tensor, nc.gpsimd, nc.sync

        bc_reg = nc.gpsimd.to_reg(NE * 128 - 1)

        def sb(stack, name, shape, dt):
            return stack.enter_context(nc.sbuf_tensor("sb_" + name, list(shape), dt))

        ps = [es.enter_context(nc.psum_tensor(f"ps{i}", [128, 512], F32)) for i in range(8)]
        ident_f = sb(es, "ident_f", [128, 128], F32)
        ident_b = sb(es, "ident_b", [128, 128], BF16)
        P.op(SP, lambda: sp.dma_start(out=ident_f[:], in_=id_d[:, :]), w=['ident_f'], dma='ident_f')
        P.op(V, lambda: ve.tensor_copy(out=ident_b[:], in_=ident_f[:]), r=['ident_f'], w=['ident_b'])
        ksum = sb(es, "ksum", [128, 4, 16], F32)
        cmask = sb(es, "cmask", [128, 4, 512], BF16)
        P.op(G, lambda: gp.dma_start(out=cmask[:], in_=cmask_d[:, :, :]), w=['cmask'], dma='cmask')

        zt = sb(es, "zt", [128, 4, D], BF16)
        P.op(G, lambda: gp.memset(zt[:], 0.0), w=['zt'])
        for a in range(NSLOT):
            P.op(G, lambda a=a: gp.dma_start(out=xs_s[a * SLOT:(a + 1) * SLOT, :].rearrange("(a p) d -> p a d", p=128), in_=zt[:]),
                 r=['zt'], w=['xs_zero'], dma='zt_st', waw=False)

        with ExitStack() as ph:
            hT = sb(ph, "hT", [128, 8, S], BF16)
            ph1 = ExitStack()
            g1 = sb(ph1, "g1", [128, D], F32)
            xt = [sb(ph1, f"xt{i}", [128, D], F32) for i in range(2)]
            hb = [sb(ph1, f"hb{i}", [128, D], BF16) for i in range(2)]
            sq = sb(ph1, "sq", [128, D], BF16)
            ss = [sb(ph1, f"ss{i}", [128, 1], F32) for i in range(2)]
            rs = [sb(ph1, f"rs{i}", [128, 1], F32) for i in range(2)]
            P.op(SP, lambda: sp.dma_start(out=g1[:], in_=g1_d[:, :].partition_broadcast(128)), w=['g1'], dma='g1')
            a1_pending = []

            def a1_copy(t, b, pb):
                P.op(A, lambda: ac.activation(out=hT[:, :, t * 128:(t + 1) * 128], in_=pb.rearrange("p (k c) -> p k c", k=8), func=AF.Copy),
                     r=[f'ps{b}'], w=[f'hT{t // 4}'], waw=False)

            for t in range(NT):
                b = t % 2
                P.op(SP, lambda t=t, b=b: sp.dma_start(out=xt[b][:], in_=x_d[t * 128:(t + 1) * 128, :]),
                     w=[f'xt{b}'], dma=f'xt{b}')
                P.op(A, lambda b=b: ac.activation(out=sq[:], in_=xt[b][:], func=AF.Square, accum_out=ss[b][:]),
                     r=[f'xt{b}'], w=['sq', f'ss{b}'])
                P.op(A, lambda b=b: ac.activation(out=rs[b][:], in_=ss[b][:], func=AF.Ln, scale=1.0 / D, bias=EPS),
                     r=[f'ss{b}'], w=[f'rs{b}'])
                P.op(A, lambda b=b: ac.activation(out=rs[b][:], in_=rs[b][:], func=AF.Exp, scale=-0.5),
                     r=[f'rs{b}'], w=[f'rs{b}'])
                P.op(V, lambda b=b: ve.scalar_tensor_tensor(out=hb[b][:], in0=xt[b][:], scalar=rs[b][:, 0:1], in1=g1[:],
                                                            op0=ALU.mult, op1=ALU.mult),
                     r=[f'xt{b}', f'rs{b}', 'g1'], w=[f'hb{b}'])
                pb = ps[b][:].bitcast(BF16)
                for kc in range(8):
                    P.op(T, lambda b=b, kc=kc, pb=pb: pe.transpose(out=pb[:, kc * 128:(kc + 1) * 128],
                                                                 in_=hb[b][:, kc * 128:(kc + 1) * 128], identity=ident_b[:]),
                         r=[f'hb{b}', 'ident_b'], w=[f'ps{b}'], waw=(kc == 0))
                a1_pending.append((t, b, pb))
                if len(a1_pending) > 1:
                    a1_copy(*a1_pending.pop(0))
            while a1_pending:
                a1_copy(*a1_pending.pop(0))
            P.flush(barrier=True)
            ph1.close()
            chk('A1')

            with ExitStack() as ph2:
                cs1 = sb(ph2, "cs1", [128, S], F32)
                cs2 = sb(ph2, "cs2", [128, S], F32)
                P.op(SP, lambda: sp.dma_start(out=cs1[:], in_=cs1_d[:, :]), w=['cs1'], dma='cs1')
                P.op(SP, lambda: sp.dma_start(out=cs2[:], in_=cs2_d[:, :]), w=['cs2'], dma='cs2')
                wb = [sb(ph2, f"wb{i}", [128, 8, 128], BF16) for i in range(4)]
                wv = [sb(ph2, f"wv{i}", [128, 8, 512], BF16) for i in range(2)]
                wfl = sb(ph2, "wfl", [128, 8, 8], BF16)
                bfg = sb(ph2, "bfg", [8, 1], F32)
                stg = [sb(ph2, f"stg{i}", [128, 512], BF16) for i in range(4)]
                t1 = [sb(ph2, f"t1_{i}", [128, 512], F32) for i in range(2)]
                t2 = [sb(ph2, f"t2_{i}", [128, 512], F32) for i in range(2)]
                vst = [sb(ph2, f"vst{i}", [128, 8, 65], BF16) for i in range(2)]
                ones8 = sb(ph2, "ones8", [8, 512], F32)
                onesb = sb(ph2, "onesb", [8, 512], BF16)
                sp8 = sb(ph2, "sp8", [8, 512], F32)
                cn8 = sb(ph2, "cn8", [8, S], F32)
                c_hi = sb(ph2, "c_hi", [8, 512], BF16)
                c_mid = sb(ph2, "c_mid", [8, 512], BF16)
                c_lo = sb(ph2, "c_lo", [8, 512], BF16)
                c_r = sb(ph2, "c_r", [8, 512], F32)
                c_t = sb(ph2, "c_t", [8, 512], F32)
                nstg = [0]
                psn = [0]

                def next_ps():
                    i = 2 + (psn[0] % 6)
                    psn[0] += 1
                    return i

                P.op(V, lambda: ve.memset(ones8[:], 1.0), w=['ones8'])
                P.op(V, lambda: ve.memset(onesb[:], 1.0), w=['onesb'])
                for i in range(2):
                    P.op(V, lambda i=i: ve.memset(vst[i][:], 1.0), w=[f'vst{i}'])
                P.op(SP, lambda: sp.dma_start(out=bfg[:], in_=bf_d[:, :]), w=['bfg'], dma='bfg')
                P.op(G, lambda: gp.dma_start(out=wfl[:], in_=wfl_d[:, :, :]), w=['wfl'], dma='wfl')
                for j in range(3):
                    for g in range(NG):
                        P.op(SP, lambda j=j, g=g: sp.dma_start(out=kf_s[:, 64 + j, g * 512:(g + 1) * 512], in_=onesb[:]),
                             r=['onesb'], w=[f'kfaug{j}_{g}'], dma='onesb_st')

                chk('A2a0')
                for g in range(NG):
                    pi = next_ps()
                    sl = slice(g * 512, (g + 1) * 512)
                    for kc in range(8):
                        P.op(T, lambda g=g, kc=kc, pi=pi: pe.matmul(ps[pi][0:8, :], lhsT=wfl[:, kc, :],
                                                                     rhs=hT[:, kc, g * 512:(g + 1) * 512],
                                                                     start=(kc == 0), stop=(kc == 7)),
                             r=['wfl', f'hT{g}'], w=[f'ps{pi}'], waw=(kc == 0))
                    P.op(V, lambda pi=pi: ve.tensor_scalar(out=sp8[:], in0=ps[pi][0:8, :],
                                                           scalar1=bfg[:, 0:1], scalar2=-1.0, op0=ALU.add, op1=ALU.mult),
                         r=[f'ps{pi}', 'bfg'], w=['sp8'])
                    P.op(A, lambda: ac.activation(out=sp8[:], in_=sp8[:], func=AF.Exp), r=['sp8'], w=['sp8'])
                    P.op(A, lambda: ac.activation(out=sp8[:], in_=sp8[:], func=AF.Ln, bias=1.0), r=['sp8'], w=['sp8'])
                    P.op(V, lambda g=g, sl=sl: ve.tensor_tensor_scan(out=cn8[:, sl], data0=ones8[:], data1=sp8[:],
                                                                     initial=(0.0 if g == 0 else cn8[:, g * 512 - 1:g * 512]),
                                                                     op0=ALU.mult, op1=ALU.add),
                         r=['sp8', 'ones8'] + ([f'cn8_{g - 1}'] if g else []), w=[f'cn8_{g}'])
                    if g == 0:
                        chk('A2a1')
                    P.op(V, lambda sl=sl: ve.tensor_scalar(out=c_t[:], in0=cn8[:, sl], scalar1=-1.0, scalar2=None, op0=ALU.mult),
                         r=[f'cn8_{g}'], w=['c_t'])
                    P.op(V, lambda: ve.tensor_copy(out=c_hi[:], in_=c_t[:]), r=['c_t'], w=['c_hi'])
                    P.op(V, lambda: ve.tensor_tensor(out=c_r[:], in0=c_t[:], in1=c_hi[:], op=ALU.subtract),
                         r=['c_t', 'c_hi'], w=['c_r'])
                    P.op(V, lambda: ve.tensor_copy(out=c_mid[:], in_=c_r[:]), r=['c_r'], w=['c_mid'])
                    P.op(V, lambda: ve.tensor_tensor(out=c_t[:], in0=c_r[:], in1=c_mid[:], op=ALU.subtract),
                         r=['c_r', 'c_mid'], w=['c_t'])
                    P.op(V, lambda: ve.tensor_copy(out=c_lo[:], in_=c_t[:]), r=['c_t'], w=['c_lo'])
                    for j, (nm, tl) in enumerate((('c_hi', c_hi), ('c_mid', c_mid), ('c_lo', c_lo))):
                        P.op(SP, lambda j=j, tl=tl, sl=sl: sp.dma_start(out=qf_s[:, 64 + j, sl], in_=tl[:]), r=[nm],
                             w=[f'qfaug{j}_{g}'], dma=f'{nm}_st')
                    if g == 0:
                        chk('A2a2')
                allc = [f'cn8_{g}' for g in range(NG)]
                P.op(SP, lambda: sp.dma_start(out=cneg_s[:, :], in_=cn8[:]), r=allc, w=['cneg_s'], dma='cn8_st')

                ccol = sb(ph2, "ccol", [128, NT * 8], F32)
                pcc = next_ps()
                for t in range(NT):
                    P.op(T, lambda t=t: pe.transpose(out=ps[pcc][:, t * 8:(t + 1) * 8], in_=cn8[:, t * 128:(t + 1) * 128],
                                                     identity=ident_f[0:8, 0:8]),
                         r=allc + ['ident_f'], w=[f'ps{pcc}'], waw=(t == 0))
                P.op(V, lambda: ve.tensor_copy(out=ccol[:], in_=ps[pcc][:, 0:NT * 8]), r=[f'ps{pcc}'], w=['ccol'])
                P.op(SP, lambda: sp.dma_start(out=ccol_s[:, :], in_=ccol[:]), r=['ccol'], w=['ccol_s'], dma='ccol_st')
                chk('A2a')
                nwb = [0]

                def load_w(blk):
                    i = nwb[0] % 4
                    nwb[0] += 1
                    P.op(G, lambda: gp.dma_start(out=wb[i][:], in_=wfm_d[blk, :, :, :]), w=[f'wb{i}'], dma=f'wb{i}')
                    return i

                def mm_block(wi, g, pi):
                    for kc in range(8):
                        P.op(T, lambda kc=kc: pe.matmul(ps[pi][:], lhsT=wb[wi][:, kc, :], rhs=hT[:, kc, g * 512:(g + 1) * 512],
                                                        start=(kc == 0), stop=(kc == 7)),
                             r=[f'wb{wi}', f'hT{g}'], w=[f'ps{pi}'], waw=(kc == 0))

                def store_pair(si, dst, pair, g):
                    for hh in range(2):
                        P.op(SP, lambda hh=hh: sp.dma_start(out=dst[2 * pair + hh, 0:64, g * 512:(g + 1) * 512],
                                                            in_=stg[si][hh * 64:(hh + 1) * 64, :]),
                             r=[f'stg{si}'], w=[f'{dst.name}_{pair}_{g}_{hh}'], dma=f'stg{si}_st')

                for blk in range(8):
                    wi = load_w(blk)
                    for g in range(NG):
                        pi = next_ps()
                        mm_block(wi, g, pi)
                        si = nstg[0] % 4
                        nstg[0] += 1
                        P.op(A, lambda pi=pi, si=si, blk=blk: ac.activation(out=stg[si][:], in_=ps[pi][:], func=AF.Identity,
                                                                            scale=(0.125 if blk < 4 else 1.0)),
                             r=[f'ps{pi}'], w=[f'stg{si}'])
                        store_pair(si, qf_s if blk < 4 else kf_s, blk % 4, g)
                chk('A2b')
                for (b0, dst, isk) in ((16, km_s, True), (8, qm_s, False)):
                    for pair in range(4):
                        w0 = load_w(b0 + pair)
                        w1 = load_w(b0 + 4 + pair)
                        for g in range(NG):
                            pa = next_ps()
                            mm_block(w0, g, pa)
                            pb_ = next_ps()
                            mm_block(w1, g, pb_)
                            ti = g % 2
                            sl = slice(g * 512, (g + 1) * 512)
                            P.op(V, lambda pa=pa, ti=ti, sl=sl: ve.tensor_tensor(out=t1[ti][:], in0=ps[pa][:], in1=cs1[:, sl], op=ALU.mult),
                                 r=[f'ps{pa}', 'cs1'], w=[f't1_{ti}'])
                            P.op(V, lambda pb_=pb_, ti=ti, sl=sl: ve.tensor_tensor(out=t2[ti][:], in0=ps[pb_][:], in1=cs2[:, sl], op=ALU.mult),
                                 r=[f'ps{pb_}', 'cs2'], w=[f't2_{ti}'])
                            si = nstg[0] % 4
                            nstg[0] += 1
                            if isk:
                                P.op(V, lambda ti=ti: ve.tensor_tensor(out=t1[ti][:], in0=t1[ti][:], in1=t2[ti][:], op=ALU.add),
                                     r=[f't1_{ti}', f't2_{ti}'], w=[f't1_{ti}'])
                                P.op(A, lambda ti=ti, si=si: ac.activation(out=stg[si][:], in_=t1[ti][:], func=AF.Copy),
                                     r=[f't1_{ti}'], w=[f'stg{si}'])
                                P.op(V, lambda ti=ti, pair=pair, g=g: ve.tensor_reduce(
                                    out=ksum[:, pair, 2 * g:2 * g + 2], in_=t1[ti][:].rearrange("p (b s) -> p b s", b=2),
                                    axis=AX.X, op=ALU.add), r=[f't1_{ti}'], w=[f'ksum_{pair}_{g}'])
                            else:
                                P.op(V, lambda ti=ti, si=si: ve.tensor_tensor(out=stg[si][:], in0=t1[ti][:], in1=t2[ti][:], op=ALU.add),
                                     r=[f't1_{ti}', f't2_{ti}'], w=[f'stg{si}'])
                            store_pair(si, dst, pair, g)
                chk('A2c')
                for blk in range(24, 40):
                    wi = load_w(blk)
                    dst = sga_s if blk < 32 else sgb_s
                    ch = (blk - 24) % 8
                    for g in range(NG):
                        pi = next_ps()
                        mm_block(wi, g, pi)
                        si = nstg[0] % 4
                        nstg[0] += 1
                        P.op(A, lambda pi=pi, si=si: ac.activation(out=stg[si][:], in_=ps[pi][:], func=AF.Sigmoid),
                             r=[f'ps{pi}'], w=[f'stg{si}'])
                        P.op(SP, lambda si=si, dst=dst, ch=ch, g=g: sp.dma_start(out=dst[ch, :, g * 512:(g + 1) * 512], in_=stg[si][:]),
                             r=[f'stg{si}'], w=[f'{dst.name}_{ch}_{g}'], dma=f'stg{si}_st')
                chk('A2d')
                nv = 0
                for vi, dst in enumerate((vf_s, vm_s)):
                    P.op(G, lambda vi=vi: gp.dma_start(out=wv[vi][:], in_=wtm_d[vi, :, :, :]), w=[f'wv{vi}'], dma=f'wv{vi}')
                    for t in range(NT):
                        pi = next_ps()
                        for kc in range(8):
                            P.op(T, lambda kc=kc, t=t, pi=pi, vi=vi: pe.matmul(ps[pi][:], lhsT=hT[:, kc, t * 128:(t + 1) * 128],
                                                                              rhs=wv[vi][:, kc, :], start=(kc == 0), stop=(kc == 7)),
                                 r=[f'wv{vi}', f'hT{t // 4}'], w=[f'ps{pi}'], waw=(kc == 0))
                        vb = nv % 2
                        nv += 1
                        P.op(A, lambda pi=pi, vb=vb: ac.activation(out=vst[vb][:, :, 0:64],
                                                                   in_=ps[pi][:].rearrange("p (h c) -> p h c", h=8), func=AF.Copy),
                             r=[f'ps{pi}'], w=[f'vst{vb}'])
                        P.op(SP, lambda vb=vb, dst=dst, t=t: sp.dma_start(out=dst[:, :, t, :].rearrange("h p c -> p h c"), in_=vst[vb][:]),
                             r=[f'vst{vb}'], w=[f'{dst.name}_{t}'], dma=f'vst{vb}_st')
                P.flush(barrier=True)

        chk('A2')
        with ExitStack() as ph:
            b1h = sb(ph, "b1h", [16, S], BF16)
            P.op(G, lambda: gp.dma_start(out=b1h[:], in_=blk1h_d[:, :]), w=['b1h'], dma='b1h')
            for h in range(NH):
                P.op(SP, lambda h=h: sp.dma_start(out=km_s[h, 64:80, :], in_=b1h[:]), r=['b1h'], w=[f'kmaug{h}'], dma='b1h_st')
            ksb = sb(ph, "ksb", [128, 4, 16], BF16)
            P.op(V, lambda: ve.tensor_copy(out=ksb[:], in_=ksum[:]), r=[f'ksum_{p_}_{g_}' for p_ in range(4) for g_ in range(NG)], w=['ksb'])
            qp = [sb(ph, f"qp{i}", [64, 2, 512], BF16) for i in range(4)]
            ksbB = sb(ph, "ksbB", [64, 4, 16], BF16)
            P.op(SP, lambda: sp.dma_start(out=ksbB[:], in_=ksb[64:128, :, :]), r=['ksb'], w=['ksbB'], dma='ksbB')
            gsb = sb(ph, "gsb", [128, 4, 8, 16], F32)
            sbt = sb(ph, "sbt", [128, 4, 8, 16], F32)
            sbb = sb(ph, "sbb", [128, 4, 128], BF16)
            tmp = sb(ph, "tmp", [128, 8, 16], F32)
            top = sb(ph, "top", [128, 8, 8], F32)
            selm = sb(ph, "selm", [128, 8, 16], F32)
            stT = [sb(ph, f"stT{i}", [128, 512], BF16) for i in range(2)]
            nq = 0
            for g in range(NG):
                sl = slice(g * 512, (g + 1) * 512)
                for pair in range(4):
                    qi = nq % 4
                    nq += 1
                    for hh in range(2):
                        P.op(SP, lambda qi=qi, hh=hh, pair=pair, sl=sl: sp.dma_start(out=qp[qi][:, hh, :],
                                                                                      in_=qm_s[2 * pair + hh, 0:64, sl]),
                             w=[f'qp{qi}'], dma=f'qp{qi}', waw=False)
                    for i in range(4):
                        for hh in range(2):
                            hg = 2 * pair + hh
                            P.op(T, lambda qi=qi, hh=hh, i=i, hg=hg, pair=pair: pe.matmul(
                                ps[0][:, i * 128 + hg * 16:i * 128 + hg * 16 + 16],
                                lhsT=qp[qi][:, hh, i * 128:(i + 1) * 128],
                                rhs=(ksb[0:64, pair, :] if hh == 0 else ksbB[:, pair, :]), start=True, stop=True),
                                 r=[f'qp{qi}', 'ksb', 'ksbB'], w=['ps0'], waw=(pair == 0 and i == 0 and hh == 0))
                P.op(V, lambda: ve.tensor_copy(out=gsb[:].rearrange("p a h n -> p (a h n)"), in_=ps[0][:]), r=['ps0'], w=['gsb'])
                P.op(V, lambda: ve.memset(sbt[:], NEG), r=[], w=['sbt'])
                for i in range(4):
                    tt = 4 * g + i
                    nb = tt // 2
                    if nb <= 3:
                        P.op(V, lambda i=i, nb=nb: ve.memset(sbt[:, i, :, 0:nb + 1], 0.0), w=['sbt'])
                        continue
                    P.op(V, lambda: ve.memset(tmp[:], -1e30), w=['tmp'])
                    P.op(V, lambda i=i, nb=nb: ve.tensor_copy(out=tmp[:, :, 0:nb], in_=gsb[:, i, :, 0:nb]), r=['gsb'], w=['tmp'])
                    for h in range(NH):
                        P.op(V, lambda h=h: ve.max(out=top[:, h, :], in_=tmp[:, h, :]), r=['tmp'], w=['top'], waw=(h == 0))
                    P.op(V, lambda: ve.tensor_tensor(out=selm[:], in0=tmp[:], in1=top[:, :, 2:3].to_broadcast([128, 8, 16]), op=ALU.is_ge),
                         r=['tmp', 'top'], w=['selm'])
                    P.op(V, lambda i=i: ve.tensor_scalar(out=sbt[:, i, :, :], in0=selm[:], scalar1=-NEG, scalar2=NEG,
                                                         op0=ALU.mult, op1=ALU.add), r=['selm'], w=['sbt'])
                    P.op(V, lambda i=i, nb=nb: ve.memset(sbt[:, i, :, nb:nb + 1], 0.0), w=['sbt'])
                P.op(V, lambda: ve.tensor_copy(out=sbb[:].rearrange("p a c -> p (a c)"), in_=sbt[:].rearrange("p a h n -> p (a h n)")),
                     r=['sbt'], w=['sbb'])
                pb1 = ps[1][:].bitcast(BF16)
                for i in range(4):
                    P.op(T, lambda i=i, pb1=pb1: pe.transpose(out=pb1[:, i * 128:(i + 1) * 128], in_=sbb[:, i, :], identity=ident_b[:]),
                         r=['sbb', 'ident_b'], w=['ps1'], waw=(i == 0))
                si = g % 2
                P.op(A, lambda si=si, pb1=pb1: ac.activation(out=stT[si][:], in_=pb1[:, 0:512], func=AF.Copy), r=['ps1'], w=[f'stT{si}'])
                for h in range(NH):
                    P.op(SP, lambda h=h, si=si, sl=sl: sp.dma_start(out=qm_s[h, 64:80, sl], in_=stT[si][h * 16:(h + 1) * 16, :]),
                         r=[f'stT{si}'], w=[f'qmaug{h}_{g}'], dma=f'stT{si}_st')
            P.flush(barrier=True)
        chk('A3')

        with ExitStack() as ph:
            qh = [sb(ph, f"qh{i}", [KM, S], BF16) for i in range(2)]
            kh = [sb(ph, f"kh{i}", [KM, S], BF16) for i in range(2)]
            vh = [sb(ph, f"vh{i}", [128, NT, 65], BF16) for i in range(2)]
            ccl = sb(ph, "ccl", [128, NT * 8], F32)
            pT = [sb(ph, f"pT{i}", [128, 512], BF16) for i in range(4)]
            osb = [sb(ph, f"osb{i}", [65, 512], F32) for i in range(2)]
            ysb = [sb(ph, f"ysb{i}", [64, 512], BF16) for i in range(2)]
            sel65 = sb(ph, "sel65", [65, 128], F32)
            P.op(V, lambda: ve.memset(sel65[:], 0.0), w=['sel65'])
            P.op(V, lambda: ve.memset(sel65[64:65, :], 1.0), w=['sel65'])
            P.op(SP, lambda: sp.dma_start(out=ccl[:], in_=ccol_s[:, :]), w=['ccl'], dma='ccl')
            cfgs = ((qf_s, kf_s, vf_s, yf_s, KF), (qm_s, km_s, vm_s, ym_s, KM))
            heads = [(br, h) for br in range(2) for h in range(NH)]
            if stop == 'B0':
                heads = heads[:1]
            elif stop == 'B':
                heads = heads[:NH]

            def emit_loads(idx):
                br, h = heads[idx]
                q_s, k_s, v_s, y_s, KK = cfgs[br]
                hb_ = idx % 2
                P.op(SP, lambda: sp.dma_start(out=qh[hb_][0:KK, :], in_=q_s[h, :, :]), w=[f'qh{hb_}'], dma=f'qh{hb_}')
                P.op(SP, lambda: sp.dma_start(out=kh[hb_][0:KK, :], in_=k_s[h, :, :]), w=[f'kh{hb_}'], dma=f'kh{hb_}')
                P.op(SP, lambda: sp.dma_start(out=vh[hb_][:], in_=v_s[h, :, :, :]), w=[f'vh{hb_}'], dma=f'vh{hb_}')

            tiles = []
            for idx, (br, h) in enumerate(heads):
                for g in range(NG):
                    for J in range(4 * g + 4):
                        tiles.append((idx, br, h, g, J))
            LA = 3
            deferred = []

            def emit_score(n):
                idx, br, h, g, J = tiles[n]
                KK = cfgs[br][4]
                hb_ = idx % 2
                pi = n % 4
                diag = J >= 4 * g
                c0 = 128 * (J - 4 * g) if diag else 0
                P.op(T, lambda: pe.matmul(ps[pi][:, c0:512], lhsT=kh[hb_][0:KK, J * 128:(J + 1) * 128], rhs=qh[hb_][0:KK, g * 512 + c0:(g + 1) * 512],
                                          start=True, stop=(not diag)), r=[f'kh{hb_}', f'qh{hb_}'], w=[f'ps{pi}'])
                if diag:
                    P.op(T, lambda: pe.matmul(ps[pi][:, c0:512], lhsT=ident_b[:], rhs=cmask[:, J - 4 * g, c0:512], start=False, stop=True),
                         r=['ident_b', 'cmask'], w=[f'ps{pi}'], waw=False)
                if br == 0:
                    P.op(A, lambda: ac.activation(out=pT[pi][:, c0:512], in_=ps[pi][:, c0:512], func=AF.Exp, bias=ccl[:, J * 8 + h:J * 8 + h + 1]),
                         r=[f'ps{pi}', 'ccl'], w=[f'pT{pi}'])
                else:
                    P.op(A, lambda: ac.activation(out=pT[pi][:, c0:512], in_=ps[pi][:, c0:512], func=AF.Exp, scale=0.125), r=[f'ps{pi}'], w=[f'pT{pi}'])

            def emit_pv(n):
                idx, br, h, g, J = tiles[n]
                y_s = cfgs[br][3]
                hb_ = idx % 2
                pi = n % 4
                gi = idx * NG + g
                po = 4 + gi % 2
                ob = gi % 2
                nj = 4 * g + 4
                c0 = 128 * (J - 4 * g) if J >= 4 * g else 0
                P.op(T, lambda: pe.matmul(ps[po][0:65, c0:512], lhsT=vh[hb_][:, J, :], rhs=pT[pi][:, c0:512], start=(J == 0), stop=(J == nj - 1)),
                     r=[f'vh{hb_}', f'pT{pi}'], w=[f'ps{po}'], waw=(J == 0))
                if J == nj - 1:
                    P.op(V, lambda: ve.tensor_copy(out=osb[ob][:], in_=ps[po][0:65, :]), r=[f'ps{po}'], w=[f'osb{ob}'])
                    P.op(V, lambda: ve.reciprocal(out=osb[ob][64:65, :], in_=osb[ob][64:65, :]), r=[f'osb{ob}'], w=[f'osb{ob}'])
                    pbc = 6 + ob

                    def stage2():
                        P.op(T, lambda: pe.matmul(ps[pbc][:], lhsT=sel65[:], rhs=osb[ob][:], start=True, stop=True),
                             r=['sel65', f'osb{ob}'], w=[f'ps{pbc}'])
                        P.op(V, lambda: ve.tensor_tensor(out=ysb[ob][:], in0=osb[ob][0:64, :], in1=ps[pbc][0:64, :], op=ALU.mult),
                             r=[f'osb{ob}', f'ps{pbc}'], w=[f'ysb{ob}'])
                        P.op(SP, lambda: sp.dma_start(out=y_s[h * 64:(h + 1) * 64, g * 512:(g + 1) * 512], in_=ysb[ob][:]),
                             r=[f'ysb{ob}'], w=[f'{y_s.name}_{h}_{g}'], dma=f'ysb{ob}_st')
                    deferred.append([n + 3, stage2])
                if g == 0 and J == 0 and idx + 1 < len(heads):
                    emit_loads(idx + 1)

            emit_loads(0)
            for n in range(len(tiles) + LA):
                if n < len(tiles):
                    emit_score(n)
                if n >= LA:
                    emit_pv(n - LA)
                    while deferred and deferred[0][0] <= n - LA:
                        deferred.pop(0)[1]()
            while deferred:
                deferred.pop(0)[1]()
            P.flush(barrier=True)
        chk('C')

        wgt = sb(es, "wgt", [128, NT, 2], F32)
        oh = sb(es, "oh", [128, 2, NT, NE], F32)
        pref = sb(es, "pref", [128, NT, NE], F32)
        run = sb(es, "run", [128, NE], F32)
        dI = sb(es, "dI", [128, 2, NT], I32)
        widx = sb(es, "widx", [128, NSLOT], I32)

        ixt = [sb(es, f"ixt{i}", [128, 1], I32) for i in range(6)]
        nix = [0]

        def idx_tile(src_ap, srcres):
            i = nix[0] % 6
            nix[0] += 1
            P.op(G, lambda: gp.tensor_copy(out=ixt[i][:], in_=src_ap), r=[srcres], w=[f'ixt{i}'])
            return ixt[i], f'ixt{i}'

        def rms_rstd(src, srcname, sqt, sst, rst, nm):
            P.op(A, lambda: ac.activation(out=sqt[:], in_=src, func=AF.Square, accum_out=sst[:]), r=[srcname], w=[nm + 'sq', nm + 'ss'])
            P.op(A, lambda: ac.activation(out=rst[:], in_=sst[:], func=AF.Ln, scale=1.0 / D, bias=EPS), r=[nm + 'ss'], w=[nm + 'rs'])
            P.op(A, lambda: ac.activation(out=rst[:], in_=rst[:], func=AF.Exp, scale=-0.5), r=[nm + 'rs'], w=[nm + 'rs'])

        with ExitStack() as ph:
            wfb = sb(ph, "wfb", [128, 4, D], BF16)
            wmb = sb(ph, "wmb", [128, 4, D], BF16)
            wo = sb(ph, "wo", [128, 8, D], BF16)
            wr = sb(ph, "wr", [128, 8, 36], F32)
            brt = sb(ph, "brt", [128, 36], F32)
            g2 = sb(ph, "g2", [128, D], F32)
            ustr = sb(ph, "ustr", [128, 128], BF16)
            onesm = sb(ph, "onesm", [128, 128], BF16)
            P.op(G, lambda: gp.dma_start(out=wfb[:], in_=wfb_d[:, :, :]), w=['wfb'], dma='wfb')
            P.op(G, lambda: gp.dma_start(out=wmb[:], in_=wmb_d[:, :, :]), w=['wmb'], dma='wmb')
            P.op(G, lambda: gp.dma_start(out=wo[:], in_=wo_d[:, :, :]), w=['wo'], dma='wo')
            P.op(G, lambda: gp.dma_start(out=ustr[:], in_=ustr_d[:, :]), w=['ustr'], dma='ustr')
            P.op(SP, lambda: sp.dma_start(out=wr[:], in_=wr_d[:, :, :]), w=['wr'], dma='wr')
            P.op(SP, lambda: sp.dma_start(out=brt[:], in_=br_d[:, :].partition_broadcast(128)), w=['brt'], dma='brt')
            P.op(SP, lambda: sp.dma_start(out=g2[:], in_=g2_d[:, :].partition_broadcast(128)), w=['g2'], dma='g2')
            P.op(V, lambda: ve.memset(onesm[:], 1.0), w=['onesm'])
            P.op(V, lambda: ve.memset(run[:], 0.0), w=['run'])
            yfT = [sb(ph, f"yfT{i}", [128, 4, 512], BF16) for i in range(2)]
            ymT = [sb(ph, f"ymT{i}", [128, 4, 512], BF16) for i in range(2)]
            sga = [sb(ph, f"sga{i}", [128, 8, 512], BF16) for i in range(2)]
            sgb = [sb(ph, f"sgb{i}", [128, 8, 512], BF16) for i in range(2)]
            mixT = sb(ph, "mixT", [128, 8, 512], BF16)
            ta = [sb(ph, f"ta{i}", [128, 512], F32) for i in range(2)]
            tb = [sb(ph, f"tb{i}", [128, 512], F32) for i in range(2)]
            xt = [sb(ph, f"dxt{i}", [128, D], F32) for i in range(2)]
            x1 = [sb(ph, f"x1_{i}", [128, D], F32) for i in range(2)]
            h2f = [sb(ph, f"h2f{i}", [128, D], F32) for i in range(2)]
            h2b = [sb(ph, f"h2b{i}", [128, D], BF16) for i in range(2)]
            h2T = [sb(ph, f"h2T{i}", [128, 8, 128], F32) for i in range(2)]
            sqd = sb(ph, "sqd", [128, D], BF16)
            ssd = [sb(ph, f"ssd{i}", [128, 1], F32) for i in range(2)]
            rsd = [sb(ph, f"rsd{i}", [128, 1], F32) for i in range(2)]
            lg = sb(ph, "lg", [128, 36], F32)
            sm = sb(ph, "sm", [128, 16], F32)
            gmask = sb(ph, "gmask", [128, 4], F32)
            flm = sb(ph, "flm", [128, 4, 8], F32)
            top8 = sb(ph, "top8", [128, 8], F32)
            gex = sb(ph, "gex", [128, 4], F32)
            mt = sb(ph, "mt", [128, NE], BF16)
            npd = 0
            d_pending = []

            def d_counts(tt):
                P.op(T, lambda: pe.matmul(ps[7][:, 0:32], lhsT=ustr[:], rhs=mt[:], start=True, stop=True), r=['ustr', 'mt'], w=['ps7'])
                P.op(T, lambda: pe.matmul(ps[7][:, 32:64], lhsT=onesm[:], rhs=mt[:], start=True, stop=True), r=['onesm', 'mt'], w=['ps7'], waw=False)
                P.op(V, lambda: ve.tensor_tensor(out=pref[:, tt, :], in0=ps[7][:, 0:32], in1=run[:], op=ALU.add), r=['ps7', 'run'], w=[f'pref{tt}'])
                P.op(V, lambda: ve.tensor_tensor(out=run[:], in0=ps[7][:, 32:64], in1=run[:], op=ALU.add), r=['ps7', 'run'], w=['run'])

            for g in range(NG):
                gb_ = g % 2
                sl = slice(g * 512, (g + 1) * 512)
                P.op(SP, lambda gb_=gb_, sl=sl: sp.dma_start(out=yfT[gb_][:], in_=yf_s[:, sl].rearrange("(k p) s -> p k s", p=128)), w=[f'yfT{gb_}'], dma=f'yfT{gb_}')
                P.op(SP, lambda gb_=gb_, sl=sl: sp.dma_start(out=ymT[gb_][:], in_=ym_s[:, sl].rearrange("(k p) s -> p k s", p=128)), w=[f'ymT{gb_}'], dma=f'ymT{gb_}')
                P.op(SP, lambda gb_=gb_, sl=sl: sp.dma_start(out=sga[gb_][:], in_=sga_s[:, :, sl].rearrange("c p s -> p c s")), w=[f'sga{gb_}'], dma=f'sga{gb_}')
                P.op(SP, lambda gb_=gb_, sl=sl: sp.dma_start(out=sgb[gb_][:], in_=sgb_s[:, :, sl].rearrange("c p s -> p c s")), w=[f'sgb{gb_}'], dma=f'sgb{gb_}')
                for c in range(8):
                    pa = (npd % 2) * 2
                    pb_ = pa + 1
                    ti = npd % 2
                    npd += 1
                    for kc in range(4):
                        P.op(T, lambda kc=kc, c=c, pa=pa, gb_=gb_: pe.matmul(ps[pa][:], lhsT=wfb[:, kc, c * 128:(c + 1) * 128], rhs=yfT[gb_][:, kc, :],
                                                                           start=(kc == 0), stop=(kc == 3)), r=['wfb', f'yfT{gb_}'], w=[f'ps{pa}'], waw=(kc == 0))
                    for kc in range(4):
                        P.op(T, lambda kc=kc, c=c, pb_=pb_, gb_=gb_: pe.matmul(ps[pb_][:], lhsT=wmb[:, kc, c * 128:(c + 1) * 128], rhs=ymT[gb_][:, kc, :],
                                                                             start=(kc == 0), stop=(kc == 3)), r=['wmb', f'ymT{gb_}'], w=[f'ps{pb_}'], waw=(kc == 0))
                    P.op(V, lambda c=c, pa=pa, ti=ti, gb_=gb_: ve.tensor_tensor(out=ta[ti][:], in0=sga[gb_][:, c, :], in1=ps[pa][:], op=ALU.mult),
                         r=[f'sga{gb_}', f'ps{pa}'], w=[f'ta{ti}'])
                    P.op(V, lambda c=c, pb_=pb_, ti=ti, gb_=gb_: ve.tensor_tensor(out=tb[ti][:], in0=sgb[gb_][:, c, :], in1=ps[pb_][:], op=ALU.mult),
                         r=[f'sgb{gb_}', f'ps{pb_}'], w=[f'tb{ti}'])
                    P.op(G, lambda c=c, ti=ti: gp.tensor_tensor(out=mixT[:, c, :], in0=ta[ti][:], in1=tb[ti][:], op=ALU.add),
                         r=[f'ta{ti}', f'tb{ti}'], w=[f'mixT{c}'])
                for i in range(4):
                    tt = 4 * g + i
                    b = tt % 2
                    P.op(SP, lambda tt=tt, b=b: sp.dma_start(out=xt[b][:], in_=x_d[tt * 128:(tt + 1) * 128, :]), w=[f'dxt{b}'], dma=f'dxt{b}')
                    for half in range(2):
                        po = 4 + half
                        for c in range(8):
                            P.op(T, lambda c=c, i=i, half=half, po=po: pe.matmul(ps[po][:], lhsT=mixT[:, c, i * 128:(i + 1) * 128],
                                                                                 rhs=wo[:, c, half * 512:(half + 1) * 512], start=(c == 0), stop=(c == 7)),
                                 r=['wo', f'mixT{c}'], w=[f'ps{po}'], waw=(c == 0))
                        P.op(V, lambda b=b, half=half, po=po: ve.tensor_tensor(out=x1[b][:, half * 512:(half + 1) * 512],
                                                                               in0=xt[b][:, half * 512:(half + 1) * 512], in1=ps[po][:], op=ALU.add),
                             r=[f'dxt{b}', f'ps{po}'], w=[f'x1_{b}'], waw=False)
                    while d_pending:
                        d_counts(d_pending.pop(0))
                    P.op(SP, lambda tt=tt, b=b: sp.dma_start(out=x1_s[tt * 128:(tt + 1) * 128, :], in_=x1[b][:]), r=[f'x1_{b}'], w=[f'x1s{tt}'], dma=f'x1_{b}_st')
                    rms_rstd(x1[b][:], f'x1_{b}', sqd, ssd[b], rsd[b], f'd{b}')
                    P.op(V, lambda b=b: ve.scalar_tensor_tensor(out=h2f[b][:], in0=x1[b][:], scalar=rsd[b][:, 0:1], in1=g2[:], op0=ALU.mult, op1=ALU.mult),
                         r=[f'x1_{b}', f'd{b}rs', 'g2'], w=[f'h2f{b}'])
                    P.op(G, lambda b=b: gp.tensor_copy(out=h2b[b][:], in_=h2f[b][:]), r=[f'h2f{b}'], w=[f'h2b{b}'])
                    P.op(SP, lambda tt=tt, b=b: sp.dma_start(out=h2_s[tt * 128:(tt + 1) * 128, :], in_=h2b[b][:]), r=[f'h2b{b}'], w=[f'h2s{tt}'], dma=f'h2b{b}_st')
                    for kc in range(8):
                        pt_ = 6 + kc // 4
                        P.op(T, lambda kc=kc, b=b, pt_=pt_: pe.transpose(out=ps[pt_][:, (kc % 4) * 128:(kc % 4 + 1) * 128], in_=h2f[b][:, kc * 128:(kc + 1) * 128],
                                                                         identity=ident_f[:]), r=[f'h2f{b}', 'ident_f'], w=[f'ps{pt_}'], waw=(kc % 4 == 0))
                    P.op(A, lambda b=b: ac.activation(out=h2T[b][:, 0:4, :], in_=ps[6][:].rearrange("p (k c) -> p k c", k=4), func=AF.Copy), r=['ps6'], w=[f'h2T{b}'], waw=False)
                    P.op(V, lambda b=b: ve.tensor_copy(out=h2T[b][:, 4:8, :], in_=ps[7][:].rearrange("p (k c) -> p k c", k=4)), r=['ps7'], w=[f'h2T{b}'], waw=False)
                    pr = 6
                    for kc in range(8):
                        P.op(T, lambda kc=kc, b=b: pe.matmul(ps[pr][:, 0:36], lhsT=h2T[b][:, kc, :], rhs=wr[:, kc, :], start=(kc == 0), stop=(kc == 7)),
                             r=[f'h2T{b}', 'wr'], w=[f'ps{pr}'], waw=(kc == 0))
                    P.op(V, lambda: ve.tensor_tensor(out=lg[:], in0=ps[pr][:, 0:36], in1=brt[:], op=ALU.add), r=[f'ps{pr}', 'brt'], w=['lg'])
                    P.op(V, lambda: ve.tensor_reduce(out=sm[:, 0:1], in_=lg[:, 0:4], axis=AX.X, op=ALU.max), r=['lg'], w=['sm0'])
                    P.op(V, lambda: ve.tensor_scalar(out=sm[:, 1:2], in0=sm[:, 0:1], scalar1=-1.0, scalar2=None, op0=ALU.mult), r=['sm0'], w=['sm1'])
                    P.op(A, lambda: ac.activation(out=gex[:], in_=lg[:, 0:4], func=AF.Exp, bias=sm[:, 1:2], accum_out=sm[:, 2:3]), r=['lg', 'sm1'], w=['gex', 'sm2'])
                    P.op(V, lambda: ve.reciprocal(out=sm[:, 3:4], in_=sm[:, 2:3]), r=['sm2'], w=['sm3'])
                    P.op(V, lambda: ve.tensor_scalar(out=gmask[:], in0=lg[:, 0:4], scalar1=sm[:, 0:1], scalar2=None, op0=ALU.is_ge), r=['lg', 'sm0'], w=['gmask'])
                    P.op(V, lambda: ve.tensor_scalar(out=gmask[:], in0=gmask[:], scalar1=1e30, scalar2=-1e30, op0=ALU.mult, op1=ALU.add), r=['gmask'], w=['gmask'])
                    P.op(V, lambda: ve.tensor_tensor(out=flm[:], in0=lg[:, 4:36].rearrange("p (g e) -> p g e", g=4),
                                                     in1=gmask[:].unsqueeze(2).to_broadcast([128, 4, 8]), op=ALU.add), r=['lg', 'gmask'], w=['flm'])
                    P.op(V, lambda: ve.max(out=top8[:], in_=flm[:].rearrange("p g e -> p (g e)")), r=['flm'], w=['top8'])
                    for k in range(2):
                        P.op(V, lambda k=k, tt=tt: ve.tensor_scalar(out=oh[:, k, tt, :], in0=flm[:].rearrange("p g e -> p (g e)"), scalar1=top8[:, k:k + 1],
                                                                    scalar2=None, op0=ALU.is_equal), r=['flm', 'top8'], w=[f'oh{k}_{tt}'])
                    P.op(V, lambda: ve.tensor_scalar(out=sm[:, 4:5], in0=top8[:, 0:1], scalar1=-1.0, scalar2=None, op0=ALU.mult), r=['top8'], w=['sm4'])
                    P.op(A, lambda: ac.activation(out=sm[:, 5:6], in_=top8[:, 1:2], func=AF.Exp, bias=sm[:, 4:5]), r=['top8', 'sm4'], w=['sm5'])
                    P.op(V, lambda: ve.tensor_scalar(out=sm[:, 6:7], in0=sm[:, 5:6], scalar1=1.0, scalar2=None, op0=ALU.add), r=['sm5'], w=['sm6'])
                    P.op(V, lambda: ve.reciprocal(out=sm[:, 7:8], in_=sm[:, 6:7]), r=['sm6'], w=['sm7'])
                    P.op(V, lambda tt=tt: ve.tensor_tensor(out=wgt[:, tt, 0:1], in0=sm[:, 3:4], in1=sm[:, 7:8], op=ALU.mult), r=['sm3', 'sm7'], w=[f'wgt0_{tt}'])
                    P.op(V, lambda tt=tt: ve.tensor_tensor(out=wgt[:, tt, 1:2], in0=wgt[:, tt, 0:1], in1=sm[:, 5:6], op=ALU.mult), r=[f'wgt0_{tt}', 'sm5'], w=[f'wgt1_{tt}'])
                    P.op(V, lambda tt=tt: ve.tensor_tensor(out=mt[:], in0=oh[:, 0, tt, :], in1=oh[:, 1, tt, :], op=ALU.add), r=[f'oh0_{tt}', f'oh1_{tt}'], w=['mt'])
                    d_pending.append(tt)
            while d_pending:
                d_counts(d_pending.pop(0))
            P.flush(barrier=True)
        chk('D')

        with ExitStack() as ph:
            jt = sb(ph, "jt", [128, NSLOT], F32)
            pidt = sb(ph, "pidt", [128, 1], F32)
            cmp = sb(ph, "cmp", [128, NSLOT, NE], F32)
            nblk = sb(ph, "nblk", [128, NE], F32)
            pend = sb(ph, "pend", [128, NE], F32)
            pst = sb(ph, "pst", [128, NE], F32)
            ones32 = sb(ph, "ones32", [128, NE], F32)
            tA = sb(ph, "tA", [128, NT, NE], F32)
            tB = sb(ph, "tB", [128, NT, NE], F32)
            dF = sb(ph, "dF", [128, 2, NT], F32)
            ebf = sb(ph, "ebf", [128, NSLOT], F32)
            hrow = [sb(ph, f"hrow{i}", [128, D], BF16) for i in range(2)]
            P.op(SP, lambda: sp.dma_start(out=jt[:], in_=jthr_d[:, :].partition_broadcast(128)), w=['jt'], dma='jt')
            P.op(SP, lambda: sp.dma_start(out=pidt[:], in_=pid_d[:, :]), w=['pidt'], dma='pidt')
            P.op(V, lambda: ve.memset(ones32[:], 1.0), w=['ones32'])
            P.op(V, lambda: ve.tensor_tensor(out=cmp[:, 0:NJ, :].rearrange("p j e -> p e j"), in0=run[:].unsqueeze(2).to_broadcast([128, NE, NJ]),
                                             in1=jt[:, 0:NJ].unsqueeze(1).to_broadcast([128, NE, NJ]), op=ALU.is_gt), r=['run', 'jt'], w=['cmp'])
            P.op(V, lambda: ve.tensor_reduce(out=nblk[:], in_=cmp[:, 0:NJ, :].rearrange("p j e -> p e j"), axis=AX.X, op=ALU.add), r=['cmp'], w=['nblk'])
            P.op(V, lambda: ve.tensor_scalar(out=nblk[:], in0=nblk[:], scalar1=float(SLOT), scalar2=None, op0=ALU.mult), r=['nblk'], w=['nblk'])
            P.op(V, lambda: ve.tensor_tensor_scan(out=pend[:], data0=ones32[:], data1=nblk[:], initial=0.0, op0=ALU.mult, op1=ALU.add),
                 r=['nblk', 'ones32'], w=['pend'])
            P.op(V, lambda: ve.tensor_tensor(out=pst[:], in0=pend[:], in1=nblk[:], op=ALU.subtract), r=['pend', 'nblk'], w=['pst'])
            allpref = [f'pref{t}' for t in range(NT)]
            P.op(V, lambda: ve.tensor_tensor(out=tA[:], in0=pref[:], in1=pst[:].unsqueeze(1).to_broadcast([128, NT, NE]), op=ALU.add),
                 r=allpref + ['pst'], w=['tA'])
            for k in range(2):
                P.op(V, lambda k=k: ve.tensor_tensor(out=tB[:], in0=tA[:], in1=oh[:, k, :, :], op=ALU.mult), r=['tA'] + [f'oh{k}_{t}' for t in range(NT)], w=['tB'])
                P.op(V, lambda k=k: ve.tensor_reduce(out=dF[:, k, :], in_=tB[:], axis=AX.X, op=ALU.add), r=['tB'], w=[f'dF{k}'])
            P.op(V, lambda: ve.tensor_copy(out=dI[:], in_=dF[:]), r=['dF0', 'dF1'], w=['dI'])
            P.op(V, lambda: ve.tensor_tensor(out=cmp[:], in0=pend[:].unsqueeze(1).to_broadcast([128, NSLOT, NE]),
                                             in1=jt[:].unsqueeze(2).to_broadcast([128, NSLOT, NE]), op=ALU.is_le), r=['pend', 'jt'], w=['cmp'])
            P.op(V, lambda: ve.tensor_reduce(out=ebf[:], in_=cmp[:], axis=AX.X, op=ALU.add), r=['cmp'], w=['ebf'])
            P.op(V, lambda: ve.tensor_scalar(out=ebf[:], in0=ebf[:], scalar1=float(NE), scalar2=128.0, op0=ALU.min, op1=ALU.mult), r=['ebf'], w=['ebf'])
            P.op(V, lambda: ve.tensor_scalar(out=ebf[:], in0=ebf[:], scalar1=pidt[:, 0:1], scalar2=None, op0=ALU.add), r=['ebf', 'pidt'], w=['ebf'])
            P.op(V, lambda: ve.tensor_copy(out=widx[:], in_=ebf[:]), r=['ebf'], w=['widx'])
            if 'dbgI_s' in dbg:
                P.op(SP, lambda: sp.dma_start(out=dbgI_s[:, 0:2 * NT], in_=dI[:].rearrange("p k t -> p (k t)")), r=['dI'], w=['dbgI_a'], dma='dbgI')
                P.op(SP, lambda: sp.dma_start(out=dbgI_s[:, 2 * NT:], in_=widx[:]), r=['widx'], w=['dbgI_b'], dma='dbgI')
            chk('E1a')
            for tt in range(NT):
                b = tt % 2
                P.op(SP, lambda tt=tt, b=b: sp.dma_start(out=hrow[b][:], in_=h2_s[tt * 128:(tt + 1) * 128, :]), w=[f'hrow{b}'], dma=f'hrow{b}')
                for k in range(2):
                    it, itn = idx_tile(dI[:, k, tt:tt + 1], 'dI')
                    P.op(G, lambda tt=tt, b=b, k=k, it=it: gp.indirect_dma_start(out=xs_s[:, :], out_offset=bass.IndirectOffsetOnAxis(ap=it[:, :], axis=0),
                                                                                in_=hrow[b][:], in_offset=None),
                         r=[f'hrow{b}', itn], w=[f'xs_{tt}_{k}'], dma=f'hrow{b}_st')
            P.flush(barrier=True)
        chk('E1')

        with ExitStack() as ph:
            NB3 = 3
            wg = [sb(ph, f"wg{i}", [128, 8, DE], BF16) for i in range(NB3)]
            wu = [sb(ph, f"wu{i}", [128, 8, DE], BF16) for i in range(NB3)]
            wd = [sb(ph, f"wd{i}", [128, 4, D], BF16) for i in range(NB3)]
            rows = [sb(ph, f"rows{i}", [128, 4, D], BF16) for i in range(NB3)]
            rT = [sb(ph, f"rT{i}", [128, 8, SLOT], BF16) for i in range(NB3)]
            sg = [sb(ph, f"sg{i}", [128, SLOT], F32) for i in range(2)]
            aT = [sb(ph, f"aT{i}", [128, 4, SLOT], BF16) for i in range(2)]
            orow = [sb(ph, f"orow{i}", [128, 4, D], F32) for i in range(2)]
            ntr = 0
            ngu = 0
            nout = 0

            def slot_stage1(b_):
                nonlocal ntr
                s2 = b_ % NB3
                it, itn = idx_tile(widx[:, b_:b_ + 1], 'widx')
                for (wt, wdram, nm) in ((wg, wg_d, 'wg'), (wu, wu_d, 'wu'), (wd, wd_d, 'wd')):
                    P.op(G, lambda wt=wt, wdram=wdram: gp.indirect_dma_start(
                        out=wt[s2][:].rearrange("p a c -> p (a c)"), out_offset=None, in_=wdram[:, :],
                        in_offset=bass.IndirectOffsetOnAxis(ap=it[:, :], axis=0), bounds_check=bc_reg, oob_is_err=False),
                         r=[itn], w=[f'{nm}{s2}'], dma=f'{nm}{s2}')
                P.op(A, lambda: ac.dma_start(out=rows[s2][:], in_=xs_s[b_ * SLOT:(b_ + 1) * SLOT, :].rearrange("(a p) d -> p a d", p=128)),
                     w=[f'rows{s2}'], dma=f'rows{s2}')
                for a_ in range(4):
                    pt_ = ntr % 2
                    ntr += 1
                    pbt = ps[pt_][:].bitcast(BF16)
                    for kc in range(8):
                        P.op(T, lambda kc=kc, pbt=pbt, a_=a_: pe.transpose(out=pbt[:, kc * 128:(kc + 1) * 128], in_=rows[s2][:, a_, kc * 128:(kc + 1) * 128],
                                                                         identity=ident_b[:]), r=[f'rows{s2}', 'ident_b'], w=[f'ps{pt_}'], waw=(kc == 0))
                    if a_ % 2 == 0:
                        P.op(A, lambda pbt=pbt, a_=a_: ac.activation(out=rT[s2][:, :, a_ * 128:(a_ + 1) * 128], in_=pbt.rearrange("p (k c) -> p k c", k=8), func=AF.Copy),
                             r=[f'ps{pt_}'], w=[f'rT{s2}'], waw=False)
                    else:
                        P.op(V, lambda pbt=pbt, a_=a_: ve.tensor_copy(out=rT[s2][:, :, a_ * 128:(a_ + 1) * 128], in_=pbt.rearrange("p (k c) -> p k c", k=8)),
                             r=[f'ps{pt_}'], w=[f'rT{s2}'], waw=False)

            def slot_stage2(b_):
                nonlocal ngu, nout
                s2 = b_ % NB3
                so = b_ % 2
                for j in range(4):
                    pg_ = 2 + ngu % 2
                    pu_ = 4 + ngu % 2
                    ngu += 1
                    for (wt, pp_, nm) in ((wg, pg_, 'wg'), (wu, pu_, 'wu')):
                        for kc in range(8):
                            P.op(T, lambda wt=wt, pp_=pp_, kc=kc, j=j: pe.matmul(ps[pp_][:], lhsT=wt[s2][:, kc, j * 128:(j + 1) * 128], rhs=rT[s2][:, kc, :],
                                                                               start=(kc == 0), stop=(kc == 7)),
                                 r=[f'{nm}{s2}', f'rT{s2}'], w=[f'ps{pp_}'], waw=(kc == 0))
                    sgi = j % 2
                    P.op(A, lambda pg_=pg_, sgi=sgi: ac.activation(out=sg[sgi][:], in_=ps[pg_][:], func=AF.Silu), r=[f'ps{pg_}'], w=[f'sg{sgi}'])
                    P.op(V, lambda pu_=pu_, sgi=sgi, j=j: ve.tensor_tensor(out=aT[so][:, j, :], in0=sg[sgi][:], in1=ps[pu_][:], op=ALU.mult),
                         r=[f'sg{sgi}', f'ps{pu_}'], w=[f'aT{so}_{j}'])
                for a_ in range(4):
                    for half in range(2):
                        po = 6 + nout % 2
                        nout += 1
                        for j in range(4):
                            P.op(T, lambda half=half, po=po, j=j, a_=a_: pe.matmul(ps[po][:], lhsT=aT[so][:, j, a_ * 128:(a_ + 1) * 128],
                                                                                 rhs=wd[s2][:, j, half * 512:(half + 1) * 512], start=(j == 0), stop=(j == 3)),
                                 r=[f'aT{so}_{j}', f'wd{s2}'], w=[f'ps{po}'], waw=(j == 0))
                        if half == 0:
                            P.op(A, lambda po=po, a_=a_: ac.activation(out=orow[so][:, a_, 0:512], in_=ps[po][:], func=AF.Copy), r=[f'ps{po}'], w=[f'orow{so}'], waw=False)
                        else:
                            P.op(V, lambda po=po, a_=a_: ve.tensor_copy(out=orow[so][:, a_, 512:1024], in_=ps[po][:]), r=[f'ps{po}'], w=[f'orow{so}'], waw=False)
                P.op(SP, lambda: sp.dma_start(out=or_s[b_ * SLOT:(b_ + 1) * SLOT, :].rearrange("(a p) d -> p a d", p=128), in_=orow[so][:]),
                     r=[f'orow{so}'], w=[f'ors{b_}'], dma=f'orow{so}_st')

            slot_stage1(0)
            slot_stage1(1)
            for b_ in range(NSLOT):
                if b_ + 2 < NSLOT:
                    slot_stage1(b_ + 2)
                slot_stage2(b_)
            P.flush(barrier=True)
        chk('E2')

        with ExitStack() as ph:
            wpg = sb(ph, "wpg", [128, 8, D], BF16)
            wpp = sb(ph, "wpp", [128, 2, D], BF16)
            g3 = sb(ph, "g3", [128, D], F32)
            g4 = sb(ph, "g4", [128, D], F32)
            P.op(G, lambda: gp.dma_start(out=wpg[:], in_=wpg_d[:, :, :]), w=['wpg'], dma='wpg')
            P.op(G, lambda: gp.dma_start(out=wpp[:], in_=wpp_d[:, :, :]), w=['wpp'], dma='wpp')
            P.op(SP, lambda: sp.dma_start(out=g3[:], in_=g3_d[:, :].partition_broadcast(128)), w=['g3'], dma='g3')
            P.op(SP, lambda: sp.dma_start(out=g4[:], in_=g4_d[:, :].partition_broadcast(128)), w=['g4'], dma='g4')
            r1 = [sb(ph, f"r1_{i}", [128, D], F32) for i in range(2)]
            r2 = [sb(ph, f"r2_{i}", [128, D], F32) for i in range(2)]
            xa = [sb(ph, f"xa{i}", [128, D], F32) for i in range(3)]
            pt = [sb(ph, f"pt{i}", [128, PLE], F32) for i in range(2)]
            ptb = [sb(ph, f"ptb{i}", [128, PLE], BF16) for i in range(2)]
            pTt = [sb(ph, f"pTt{i}", [128, 2, 128], BF16) for i in range(2)]
            h3 = [sb(ph, f"h3_{i}", [128, D], BF16) for i in range(2)]
            h3T = [sb(ph, f"h3T{i}", [128, 8, 128], BF16) for i in range(2)]
            gs = [sb(ph, f"gs{i}", [128, D], F32) for i in range(2)]
            ot = [sb(ph, f"ot{i}", [128, D], F32) for i in range(2)]
            sqf = sb(ph, "sqf", [128, D], BF16)
            ssf = [sb(ph, f"ssf{i}", [128, 1], F32) for i in range(2)]
            rsf = [sb(ph, f"rsf{i}", [128, 1], F32) for i in range(2)]
            ssg = [sb(ph, f"ssg{i}", [128, 1], F32) for i in range(2)]
            rsg = [sb(ph, f"rsg{i}", [128, 1], F32) for i in range(2)]
            def f_stage1(tt):
                b = tt % 2
                b3 = tt % 3
                tsl = slice(tt * 128, (tt + 1) * 128)
                for k, rr in enumerate((r1, r2)):
                    it, itn = idx_tile(dI[:, k, tt:tt + 1], 'dI')
                    P.op(G, lambda b=b, it=it, rr=rr: gp.indirect_dma_start(out=rr[b][:], out_offset=None, in_=or_s[:, :],
                                                                            in_offset=bass.IndirectOffsetOnAxis(ap=it[:, :], axis=0)),
                         r=[itn], w=[f'r{k + 1}_{b}'], dma=f'r{k + 1}_{b}')
                P.op(SP, lambda tsl=tsl, b3=b3: sp.dma_start(out=xa[b3][:], in_=x1_s[tsl, :]), w=[f'xa{b3}'], dma=f'xa{b3}')
                P.op(SP, lambda tsl=tsl, b=b: sp.dma_start(out=pt[b][:], in_=p_d[tsl, :]), w=[f'pt{b}'], dma=f'pt{b}')
                P.op(V, lambda tt=tt, b=b, b3=b3: ve.scalar_tensor_tensor(out=xa[b3][:], in0=r1[b][:], scalar=wgt[:, tt, 0:1], in1=xa[b3][:], op0=ALU.mult, op1=ALU.add),
                     r=[f'r1_{b}', f'xa{b3}'], w=[f'xa{b3}'])
                P.op(V, lambda tt=tt, b=b, b3=b3: ve.scalar_tensor_tensor(out=xa[b3][:], in0=r2[b][:], scalar=wgt[:, tt, 1:2], in1=xa[b3][:], op0=ALU.mult, op1=ALU.add),
                     r=[f'r2_{b}', f'xa{b3}'], w=[f'xa{b3}'])
                rms_rstd(xa[b3][:], f'xa{b3}', sqf, ssf[b], rsf[b], f'f{b}')
                P.op(V, lambda b=b, b3=b3: ve.scalar_tensor_tensor(out=h3[b][:], in0=xa[b3][:], scalar=rsf[b][:, 0:1], in1=g3[:], op0=ALU.mult, op1=ALU.mult),
                     r=[f'xa{b3}', f'f{b}rs', 'g3'], w=[f'h3_{b}'])
                P.op(G, lambda b=b: gp.tensor_copy(out=ptb[b][:], in_=pt[b][:]), r=[f'pt{b}'], w=[f'ptb{b}'])

            def f_stageT(tt):
                b = tt % 2
                pbt = ps[b][:].bitcast(BF16)
                for kc in range(8):
                    P.op(T, lambda kc=kc, b=b, pbt=pbt: pe.transpose(out=pbt[:, kc * 128:(kc + 1) * 128], in_=h3[b][:, kc * 128:(kc + 1) * 128], identity=ident_b[:]),
                         r=[f'h3_{b}', 'ident_b'], w=[f'ps{b}'], waw=(kc == 0))
                P.op(A, lambda b=b, pbt=pbt: ac.activation(out=h3T[b][:], in_=pbt.rearrange("p (k c) -> p k c", k=8), func=AF.Copy), r=[f'ps{b}'], w=[f'h3T{b}'])
                pb2 = ps[2 + b][:].bitcast(BF16)
                for kc in range(2):
                    P.op(T, lambda kc=kc, b=b, pb2=pb2: pe.transpose(out=pb2[:, kc * 128:(kc + 1) * 128], in_=ptb[b][:, kc * 128:(kc + 1) * 128], identity=ident_b[:]),
                         r=[f'ptb{b}', 'ident_b'], w=[f'ps{2 + b}'], waw=(kc == 0))
                P.op(V, lambda b=b, pb2=pb2: ve.tensor_copy(out=pTt[b][:], in_=pb2[:, 0:256].rearrange("p (k c) -> p k c", k=2)), r=[f'ps{2 + b}'], w=[f'pTt{b}'])


            def f_stage2(tt):
                b = tt % 2
                b3 = tt % 3
                tsl = slice(tt * 128, (tt + 1) * 128)
                for half in range(2):
                    hs = slice(half * 512, (half + 1) * 512)
                    pgt = 4 + half
                    ppp = 6 + half
                    for kc in range(8):
                        P.op(T, lambda kc=kc, b=b, hs=hs, pgt=pgt: pe.matmul(ps[pgt][:], lhsT=h3T[b][:, kc, :], rhs=wpg[:, kc, hs], start=(kc == 0), stop=(kc == 7)),
                             r=[f'h3T{b}', 'wpg'], w=[f'ps{pgt}'], waw=(kc == 0))
                    P.op(A, lambda b=b, hs=hs, pgt=pgt: ac.activation(out=gs[b][:, hs], in_=ps[pgt][:], func=AF.Sigmoid), r=[f'ps{pgt}'], w=[f'gs{b}'], waw=False)
                    for kc in range(2):
                        P.op(T, lambda kc=kc, b=b, hs=hs, ppp=ppp: pe.matmul(ps[ppp][:], lhsT=pTt[b][:, kc, :], rhs=wpp[:, kc, hs], start=(kc == 0), stop=(kc == 1)),
                             r=[f'pTt{b}', 'wpp'], w=[f'ps{ppp}'], waw=(kc == 0))
                    P.op(V, lambda b=b, hs=hs, ppp=ppp: ve.tensor_tensor(out=gs[b][:, hs], in0=gs[b][:, hs], in1=ps[ppp][:], op=ALU.mult),
                         r=[f'gs{b}', f'ps{ppp}'], w=[f'gs{b}'])
                P.op(V, lambda b=b, b3=b3: ve.tensor_tensor(out=xa[b3][:], in0=xa[b3][:], in1=gs[b][:], op=ALU.add), r=[f'xa{b3}', f'gs{b}'], w=[f'xa{b3}'])
                rms_rstd(xa[b3][:], f'xa{b3}', sqf, ssg[b], rsg[b], f'g{b}')
                P.op(V, lambda b=b, b3=b3: ve.scalar_tensor_tensor(out=ot[b][:], in0=xa[b3][:], scalar=rsg[b][:, 0:1], in1=g4[:], op0=ALU.mult, op1=ALU.mult),
                     r=[f'xa{b3}', f'g{b}rs', 'g4'], w=[f'ot{b}'])
                P.op(SP, lambda tsl=tsl, b=b: sp.dma_start(out=out_d[tsl, :], in_=ot[b][:]), r=[f'ot{b}'], w=[f'out{tt}'], dma=f'ot{b}_st')

            f_stage1(0)
            f_stage1(1)
            f_stageT(0)
            for tt in range(NT):
                if tt + 2 < NT:
                    f_stage1(tt + 2)
                if tt + 1 < NT:
                    f_stageT(tt + 1)
                f_stage2(tt)
            P.flush(barrier=True)

        P.flush(barrier=True)
    except _Stop:
        pass
    return nc


def _cmask():
    s_ = np.arange(128)[:, None, None]
    j_ = np.arange(4)[None, :, None]
    t_ = np.arange(512)[None, None, :]
    return np.where(t_ - s_ - 128 * j_ >= 0, 0.0, NEG).astype(np.float32)


def _host_layout(inputs):
    w_in = np.asarray(inputs['w_in'])[0]
    cols = []
    qf = np.arange(0, 512); kf = np.arange(512, 1024); vf = np.arange(1024, 1536)
    qm = np.arange(1536, 2048); km = np.arange(2048, 2560); vm = np.arange(2560, 3072)
    fl = np.arange(3072, 3080); ga = np.arange(3080, 4104); gb = np.arange(4104, 5128)

    def swap(c):
        c = c.reshape(8, 2, 32)
        return c[:, ::-1, :].reshape(-1)

    fm = np.concatenate([qf, kf, qm, swap(qm), km, swap(km), ga, gb])
    w_fm = w_in[:, fm].reshape(8, 128, 40, 128).transpose(2, 1, 0, 3)
    w_tm = np.stack([w_in[:, vf], w_in[:, vm]]).reshape(2, 8, 128, 512).transpose(0, 2, 1, 3)
    w_fl = w_in[:, fl].reshape(8, 128, 8).transpose(1, 0, 2)
    inv = 1.0 / (10000.0 ** (np.arange(0, 64, 2, dtype=np.float32) / 64.0))
    ang = np.arange(S, dtype=np.float32)[None, :] * inv[:, None]
    cos, sin = np.cos(ang).astype(np.float32), np.sin(ang).astype(np.float32)
    cs1 = np.concatenate([cos, cos, cos, cos], 0)
    cs2 = np.concatenate([-sin, sin, -sin, sin], 0)
    shared = dict(
        w_fm=np.ascontiguousarray(w_fm, dtype=np.float32), w_tm=np.ascontiguousarray(w_tm, dtype=np.float32),
        w_fl=np.ascontiguousarray(w_fl, dtype=np.float32),
        attn_norm=np.asarray(inputs['attn_norm'], dtype=np.float32).reshape(1, D),
        b_forget=np.asarray(inputs['b_forget'], dtype=np.float32).reshape(8, 1),
        cs1=np.ascontiguousarray(cs1), cs2=np.ascontiguousarray(cs2),
        ident=np.eye(128, dtype=np.float32),
        w_fb=np.ascontiguousarray(np.asarray(inputs['w_fox_branch'], np.float32)[0].reshape(4, 128, D).transpose(1, 0, 2)),
        w_mb=np.ascontiguousarray(np.asarray(inputs['w_moba_branch'], np.float32)[0].reshape(4, 128, D).transpose(1, 0, 2)),
        w_o=np.ascontiguousarray(np.asarray(inputs['w_out'], np.float32)[0].reshape(8, 128, D).transpose(1, 0, 2)),
        moe_norm=np.asarray(inputs['moe_norm'], np.float32).reshape(1, D),
        w_r=np.ascontiguousarray(np.concatenate([np.asarray(inputs['w_group'], np.float32)[0], np.asarray(inputs['w_fine'], np.float32)[0]], 1)
                                 .reshape(8, 128, 36).transpose(1, 0, 2)),
        b_r=np.concatenate([np.asarray(inputs['b_group'], np.float32)[0], np.asarray(inputs['b_fine'], np.float32)[0]]).reshape(1, 36),
        ustrict=np.triu(np.ones((128, 128), np.float32), 1),
        jthr=(float(SLOT) * np.arange(NSLOT, dtype=np.float32)).reshape(1, NSLOT),
        pid=np.arange(128, dtype=np.float32).reshape(128, 1),
        w_g=np.ascontiguousarray(np.asarray(inputs['w_gate'], np.float32)[0].reshape(NE, 8, 128, DE).transpose(0, 2, 1, 3)).reshape(NE * 128, 8 * DE),
        w_u=np.ascontiguousarray(np.asarray(inputs['w_up'], np.float32)[0].reshape(NE, 8, 128, DE).transpose(0, 2, 1, 3)).reshape(NE * 128, 8 * DE),
        w_d=np.ascontiguousarray(np.asarray(inputs['w_down'], np.float32)[0].reshape(NE, 4, 128, D).transpose(0, 2, 1, 3)).reshape(NE * 128, 4 * D),
        ple_norm=np.asarray(inputs['ple_norm'], np.float32).reshape(1, D),
        final_norm=np.asarray(inputs['final_norm'], np.float32).reshape(1, D),
        w_pg=np.ascontiguousarray(np.asarray(inputs['w_ple_gate'], np.float32)[0].reshape(8, 128, D).transpose(1, 0, 2)),
        w_pp=np.ascontiguousarray(np.asarray(inputs['w_ple_proj'], np.float32)[0].reshape(2, 128, D).transpose(1, 0, 2)),
        cmask=_cmask(), blk1h=np.kron(np.eye(16, dtype=np.float32), np.ones((1, 256), np.float32)),
    )
    return shared


def kernel(**inputs):
    shared = _host_layout(inputs)
    x = np.asarray(inputs['x'], dtype=np.float32)
    p = np.asarray(inputs['p'], dtype=np.float32)[0]
    nc = build()
    in_maps = []
    for c in range(8):
        m = dict(shared)
        m['x'] = np.ascontiguousarray(x[c])
        m['p'] = np.ascontiguousarray(p[c])
        in_maps.append(m)
    res = run_bass_kernel_spmd(nc, in_maps, core_ids=list(range(8)))
    return np.stack([r['out'] for r in res.results], 0)
```
